# Optimizing a Trainium2 kernel written in Bass

```python
import math
import jax, jax.numpy as jnp
from jax import lax
import numpy as np

D_MODEL = 2048
BATCH = 4
SEQ = 4096
DEPTH = 4

HEAD_DIM = 128
N_HEADS = D_MODEL // HEAD_DIM
N_MEM_HEADS = N_HEADS // 4
N_MIX_HEADS = N_HEADS - N_MEM_HEADS
MIX_WIDTH = N_MIX_HEADS * HEAD_DIM
MEM_WIDTH = N_MEM_HEADS * HEAD_DIM
DILATION_PAIRS = ((128, 1), (512, 4), (2048, 16))
HEADS_PER_DIL = N_MIX_HEADS // len(DILATION_PAIRS)
SWA_RADIUS = 128
N_KV_HEADS = 2
GQA_GROUP = N_MIX_HEADS // N_KV_HEADS
KV_WIDTH = N_KV_HEADS * HEAD_DIM
IN_WIDTH_A = 3 * MIX_WIDTH + MEM_WIDTH
IN_WIDTH_B = MIX_WIDTH + 2 * KV_WIDTH + MEM_WIDTH
N_MEM_TOKENS = 256
N_EXPERTS = 32
TOP_K = 4
D_EXPERT = D_MODEL // 2
EXPERT_BLOCK = 256
SWIGLU_LIMIT = 7.0
SWIGLU_ALPHA = 1.702
N_MIXERS = 2
N_LAYERS_A = (DEPTH + N_MIXERS - 1) // N_MIXERS
N_LAYERS_B = DEPTH // N_MIXERS
DN_ALPHA = (2 * DEPTH) ** 0.25
DN_BETA = (8 * DEPTH) ** -0.25
LN_EPS = 1e-5
NEG_INF = -1e30

kernel_name = "hybrid_dilated_swa_memory_moe_encoder"


def layer_norm(x, g, b):
    xf = x.astype(jnp.float32)
    mu = jnp.mean(xf, axis=-1, keepdims=True)
    var = jnp.mean(jnp.square(xf - mu), axis=-1, keepdims=True)
    return ((xf - mu) * lax.rsqrt(var + LN_EPS) * g.astype(jnp.float32) + b.astype(jnp.float32)).astype(x.dtype)


def alibi_slopes(n):
    return jnp.exp2(-8.0 * jnp.arange(1, n + 1, dtype=jnp.float32) / n)


def banded_attention(q, k, v, radius, slopes, dist_unit, sink, return_lse):
    B, L, G, R, Dh = q.shape
    W = radius
    nb = -(-L // W)
    n = nb * W
    pad = n - L
    qb = jnp.pad(q, ((0, 0), (0, pad), (0, 0), (0, 0), (0, 0))).reshape(B, nb, W, G, R, Dh)

    def neighbour_blocks(t):
        tp = jnp.pad(t, ((0, 0), (W, W + pad), (0, 0), (0, 0)))
        return jnp.concatenate([tp[:, o:o + n].reshape(B, nb, W, G, Dh) for o in (0, W, 2 * W)], axis=2)

    kb = neighbour_blocks(k)
    vb = neighbour_blocks(v)
    s = jnp.einsum('bnqgrd,bnkgd->bngrqk', qb, kb, preferred_element_type=jnp.float32) * (Dh ** -0.5)
    blk = jnp.arange(nb)[:, None, None] * W
    qpos = blk + jnp.arange(W)[None, :, None]
    kpos = blk - W + jnp.arange(3 * W)[None, None, :]
    dist = jnp.abs(qpos - kpos)
    valid = (dist <= W) & (kpos >= 0) & (kpos < L)
    slope = slopes.astype(jnp.float32) * dist_unit
    s = s - slope[:, :, None, None] * dist[:, None, None].astype(jnp.float32)
    s = jnp.where(valid[:, None, None], s, NEG_INF)
    m = jnp.max(s, axis=-1)
    if sink is not None:
        sk = sink.astype(jnp.float32)[:, :, None]
        m = jnp.maximum(m, sk)
    e = jnp.exp(s - m[..., None])
    den = jnp.sum(e, axis=-1)
    if sink is not None:
        den = den + jnp.exp(sk - m)
    o = jnp.einsum('bngrqk,bnkgd->bnqgrd', e, vb.astype(jnp.float32))
    o = o / jnp.transpose(den, (0, 1, 4, 2, 3))[..., None]
    o = o.reshape(B, n, G, R, Dh)[:, :L].astype(q.dtype)
    if not return_lse:
        return o
    lse = jnp.transpose(m + jnp.log(den), (0, 1, 4, 2, 3)).reshape(B, n, G, R)[:, :L]
    return o, lse


def dilated_group_attention(q, k, v, radius, dilation, slopes):
    B, S, H, Dh = q.shape
    L = S // dilation

    def by_residue(t):
        return t.reshape(B, L, dilation, H, Dh).transpose(0, 2, 1, 3, 4).reshape(B * dilation, L, H, Dh)

    o, lse = banded_attention(by_residue(q)[:, :, :, None], by_residue(k), by_residue(v),
                              radius, slopes[:, None], dilation, None, True)
    o = o.reshape(B, dilation, L, H, Dh).transpose(0, 2, 1, 3, 4).reshape(B, S, H, Dh)
    lse = lse.reshape(B, dilation, L, H).transpose(0, 2, 1, 3).reshape(B, S, H)
    return o, lse


def dilated_mixture(q, k, v, slopes):
    B, S = q.shape[:2]
    outs, lses = [], []
    for g, (window, dilation) in enumerate(DILATION_PAIRS):
        sl = slice(g * HEADS_PER_DIL, (g + 1) * HEADS_PER_DIL)
        o, l = dilated_group_attention(q[:, :, sl], k[:, :, sl], v[:, :, sl],
                                       window // (2 * dilation), dilation, slopes[sl])
        outs.append(o)
        lses.append(l)
    wts = jax.nn.softmax(jnp.stack(lses, axis=0), axis=0)
    mixed = jnp.concatenate([o * wts[g][..., None].astype(o.dtype) for g, o in enumerate(outs)], axis=2)
    return mixed.reshape(B, S, MIX_WIDTH)


def windowed_gqa(q, k, v, slopes, sink):
    B, S = q.shape[:2]
    o = banded_attention(q, k, v, SWA_RADIUS, slopes.reshape(N_KV_HEADS, GQA_GROUP), 1,
                         sink.reshape(N_KV_HEADS, GQA_GROUP), False)
    return o.reshape(B, S, MIX_WIDTH)


def memory_attention(q, mem_k, mem_v):
    B, S = q.shape[:2]
    s = jnp.einsum('bshd,bmhd->bhsm', q, mem_k, preferred_element_type=jnp.float32) * (HEAD_DIM ** -0.5)
    p = jax.nn.softmax(s, axis=-1)
    o = jnp.einsum('bhsm,bmhd->bshd', p, mem_v.astype(jnp.float32))
    return o.reshape(B, S, MEM_WIDTH).astype(q.dtype)


def moe_ffn(h, router_w, router_b, w_gate_up, b_gate_up, w_down, b_down):
    N, D = h.shape
    logits = jnp.einsum('nd,de->ne', h, router_w, preferred_element_type=jnp.float32) + router_b.astype(jnp.float32)
    top_val, top_idx = lax.top_k(logits, TOP_K)
    gates = jax.nn.softmax(top_val, axis=-1)
    A = N * TOP_K
    flat_e = top_idx.reshape(A)
    flat_tok = jnp.arange(A, dtype=jnp.int32) // TOP_K
    order = jnp.argsort(flat_e)
    sorted_e = flat_e[order]
    counts = jnp.bincount(flat_e, length=N_EXPERTS)
    padded = (counts + EXPERT_BLOCK - 1) // EXPERT_BLOCK * EXPERT_BLOCK
    pad_end = jnp.cumsum(padded)
    pad_start = pad_end - padded
    start = jnp.cumsum(counts) - counts
    dest = pad_start[sorted_e] + jnp.arange(A, dtype=jnp.int32) - start[sorted_e]
    n_blocks = (A + N_EXPERTS * (EXPERT_BLOCK - 1) + EXPERT_BLOCK - 1) // EXPERT_BLOCK
    P = n_blocks * EXPERT_BLOCK
    slot_tok = jnp.full((P,), N, jnp.int32).at[dest].set(flat_tok[order])
    slot_gate = jnp.zeros((P,), jnp.float32).at[dest].set(gates.reshape(A)[order])
    blk_expert = jnp.minimum(jnp.searchsorted(pad_end, jnp.arange(n_blocks) * EXPERT_BLOCK, side='right'),
                             N_EXPERTS - 1).astype(jnp.int32)
    h_pad = jnp.concatenate([h, jnp.zeros((1, D), h.dtype)], axis=0)

    def expert_block(args):
        tok, e = args
        xb = h_pad[tok]
        gu = xb @ w_gate_up[e] + b_gate_up[e]
        g = jnp.minimum(gu[:, :D_EXPERT], SWIGLU_LIMIT)
        u = jnp.clip(gu[:, D_EXPERT:], -SWIGLU_LIMIT, SWIGLU_LIMIT)
        act = g * jax.nn.sigmoid(SWIGLU_ALPHA * g) * (u + 1.0)
        return act @ w_down[e] + b_down[e]

    y = lax.map(expert_block, (slot_tok.reshape(n_blocks, EXPERT_BLOCK), blk_expert))
    out = jnp.zeros((N + 1, D), jnp.float32).at[slot_tok].add(
        y.reshape(P, D).astype(jnp.float32) * slot_gate[:, None])
    return out[:N].astype(h.dtype)


def setup_inputs(seed: int = 0) -> dict:
    key = jax.random.key(seed)
    ks = jax.random.split(key, 20)
    f32 = jnp.float32

    def nrm(k, shape, scale):
        return jax.random.normal(k, shape, f32) * scale

    d_s = D_MODEL ** -0.5
    x = nrm(ks[0], (BATCH, SEQ, D_MODEL), 1.0)
    mem = nrm(ks[1], (BATCH, N_MEM_TOKENS, D_MODEL), 1.0)
    col_a = jnp.concatenate([jnp.ones((2 * MIX_WIDTH,), f32), jnp.full((MIX_WIDTH,), DN_BETA, f32),
                             jnp.ones((MEM_WIDTH,), f32)])
    w_in_a = nrm(ks[2], (N_LAYERS_A, D_MODEL, IN_WIDTH_A), d_s) * col_a
    col_b = jnp.concatenate([jnp.ones((MIX_WIDTH + KV_WIDTH,), f32), jnp.full((KV_WIDTH,), DN_BETA, f32),
                             jnp.ones((MEM_WIDTH,), f32)])
    w_in_b = nrm(ks[3], (N_LAYERS_B, D_MODEL, IN_WIDTH_B), d_s) * col_b
    sink_b = nrm(ks[4], (N_LAYERS_B, N_MIX_HEADS), 0.5)
    col_m = jnp.concatenate([jnp.ones((MEM_WIDTH,), f32), jnp.full((MEM_WIDTH,), DN_BETA, f32)])
    w_mem_kv = nrm(ks[5], (DEPTH, D_MODEL, 2 * MEM_WIDTH), d_s) * col_m
    w_o = nrm(ks[6], (DEPTH, D_MODEL, D_MODEL), d_s * DN_BETA)
    ln1_g = 1.0 + nrm(ks[7], (DEPTH, D_MODEL), 0.02)
    ln1_b = nrm(ks[8], (DEPTH, D_MODEL), 0.02)
    router_w = nrm(ks[9], (DEPTH, D_MODEL, N_EXPERTS), d_s)
    router_b = nrm(ks[10], (DEPTH, N_EXPERTS), 0.01)
    w_gate_up = nrm(ks[11], (DEPTH, N_EXPERTS, D_MODEL, 2 * D_EXPERT), d_s)
    b_gate_up = nrm(ks[12], (DEPTH, N_EXPERTS, 2 * D_EXPERT), 0.02)
    w_down = nrm(ks[13], (DEPTH, N_EXPERTS, D_EXPERT, D_MODEL), (D_EXPERT ** -0.5) * DN_BETA)
    b_down = nrm(ks[14], (DEPTH, N_EXPERTS, D_MODEL), 0.02)
    ln2_g = 1.0 + nrm(ks[15], (DEPTH, D_MODEL), 0.02)
    ln2_b = nrm(ks[16], (DEPTH, D_MODEL), 0.02)
    return {"x": x, "mem": mem, "w_in_a": w_in_a, "w_in_b": w_in_b, "sink_b": sink_b,
            "w_mem_kv": w_mem_kv, "w_o": w_o, "ln1_g": ln1_g, "ln1_b": ln1_b,
            "router_w": router_w, "router_b": router_b, "w_gate_up": w_gate_up,
            "b_gate_up": b_gate_up, "w_down": w_down, "b_down": b_down,
            "ln2_g": ln2_g, "ln2_b": ln2_b}


def reference(x, mem, w_in_a, w_in_b, sink_b, w_mem_kv, w_o, ln1_g, ln1_b, router_w, router_b,
              w_gate_up, b_gate_up, w_down, b_down, ln2_g, ln2_b):
    B, S, D = x.shape
    M = mem.shape[1]
    slopes = alibi_slopes(N_MIX_HEADS)
    for i in range(DEPTH):
        j = i // N_MIXERS
        mkv = jnp.einsum('bmd,de->bme', mem, w_mem_kv[i])
        mem_k = mkv[..., :MEM_WIDTH].reshape(B, M, N_MEM_HEADS, HEAD_DIM)
        mem_v = mkv[..., MEM_WIDTH:].reshape(B, M, N_MEM_HEADS, HEAD_DIM)
        if i % N_MIXERS == 0:
            proj = jnp.einsum('bsd,de->bse', x, w_in_a[j])
            q = proj[..., :MIX_WIDTH].reshape(B, S, N_MIX_HEADS, HEAD_DIM)
            k = proj[..., MIX_WIDTH:2 * MIX_WIDTH].reshape(B, S, N_MIX_HEADS, HEAD_DIM)
            v = proj[..., 2 * MIX_WIDTH:3 * MIX_WIDTH].reshape(B, S, N_MIX_HEADS, HEAD_DIM)
            q_mem = proj[..., 3 * MIX_WIDTH:]
            mix = dilated_mixture(q, k, v, slopes)
        else:
            proj = jnp.einsum('bsd,de->bse', x, w_in_b[j])
            q = proj[..., :MIX_WIDTH].reshape(B, S, N_KV_HEADS, GQA_GROUP, HEAD_DIM)
            k = proj[..., MIX_WIDTH:MIX_WIDTH + KV_WIDTH].reshape(B, S, N_KV_HEADS, HEAD_DIM)
            v = proj[..., MIX_WIDTH + KV_WIDTH:MIX_WIDTH + 2 * KV_WIDTH].reshape(B, S, N_KV_HEADS, HEAD_DIM)
            q_mem = proj[..., MIX_WIDTH + 2 * KV_WIDTH:]
            mix = windowed_gqa(q, k, v, slopes, sink_b[j])
        mem_out = memory_attention(q_mem.reshape(B, S, N_MEM_HEADS, HEAD_DIM), mem_k, mem_v)
        heads = jnp.concatenate([mix, mem_out], axis=-1)
        x = layer_norm(DN_ALPHA * x + jnp.einsum('bse,ed->bsd', heads, w_o[i]), ln1_g[i], ln1_b[i])
        ffn = moe_ffn(x.reshape(B * S, D), router_w[i], router_b[i], w_gate_up[i], b_gate_up[i],
                      w_down[i], b_down[i]).reshape(B, S, D)
        x = layer_norm(DN_ALPHA * x + ffn, ln2_g[i], ln2_b[i])
    return x
```

```python
import contextlib
import numpy as np
import concourse.bass as bass
import concourse.mybir as mybir
from concourse.bass_utils import run_bass_kernel_spmd

F32 = mybir.dt.float32
BF16 = mybir.dt.bfloat16
I32 = mybir.dt.int32
U32 = mybir.dt.uint32
ALU = mybir.AluOpType
AF = mybir.ActivationFunctionType
AX = mybir.AxisListType

D = 2048
KC = 16
DEPTH = 4
NQ = [4096, 4096, 4096, 4096]
NK = [4096, 4096, 4096, 4096]
CAP = [640, 640, 640, 640]
NE = 32
FE = 1024
ALPHA = float(8.0 ** 0.25)
EPS = 1e-5
SCALE = float(128.0 ** -0.5)
DIL = (1, 4, 16)
NEG = -1e30
MAXT = 32


class Buf:
    __slots__ = ("w", "wl", "r", "rp")

    def __init__(self):
        self.w = None
        self.wl = []
        self.r = {}
        self.rp = {}


class Sched:
    def __init__(self, nc, stack, n_dma=(("sp", 32), ("act", 6), ("pool", 32))):
        self.nc = nc
        self.eng = {"pe": nc.tensor, "act": nc.scalar, "dve": nc.vector,
                    "pool": nc.gpsimd, "sp": nc.sync}
        self.csem = {}
        self.ccnt = {}
        self.ckey = {}
        self.stack = stack
        self.new_epoch("e0")
        self.dpool = {}
        self.dnext = {}
        for q, n in n_dma:
            self.dpool[q] = [[stack.enter_context(nc.semaphore("d_%s_%d" % (q, i))), 0,
                              "d_%s_%d" % (q, i)] for i in range(n)]
            self.dnext[q] = 0
        self.waited = {k: {} for k in self.eng}
        self.n_ins = 0
        self.n_wait = 0

    def new_epoch(self, tag):
        for k in ("pe", "act", "dve", "pool"):
            self.ckey[k] = "c_%s_%s" % (k, tag)
            self.csem[k] = self.stack.enter_context(self.nc.semaphore(self.ckey[k]))
            self.ccnt[k] = 0

    def _wait(self, ek, ev):
        if ev is None:
            return
        sem, val, key = ev
        if ek == "pe" and key.startswith("c_pe_"):
            return
        w = self.waited[ek]
        if w.get(key, 0) >= val:
            return
        self.eng[ek].wait_ge(sem, val)
        w[key] = val
        self.n_wait += 1

    def _deps(self, ek, reads, writes, accw):
        for b in reads:
            self._wait(ek, b.w)
            for ev in b.wl:
                self._wait(ek, ev)
        for b in writes:
            self._wait(ek, b.w)
            for ev in b.wl:
                self._wait(ek, ev)
            for ev in b.r.values():
                self._wait(ek, ev)
            for ev in b.rp.values():
                self._wait(ek, ev)
        for b in accw:
            if b.r:
                b.rp = dict(b.r)
                b.r = {}
                b.wl = []
            self._wait(ek, b.w)
            for ev in b.rp.values():
                self._wait(ek, ev)

    def _commit(self, ev, rkey, reads, writes, accw):
        for b in reads:
            b.r[rkey] = ev
        for b in writes:
            b.w = ev
            b.wl = []
            b.r = {}
            b.rp = {}
        for b in accw:
            b.wl.append(ev)

    def op(self, ek, fn, reads=(), writes=(), inc=True):
        self._deps(ek, reads, writes, ())
        ins = fn(self.eng[ek])
        if inc:
            self.ccnt[ek] += 1
            ins.then_inc(self.csem[ek], 1)
            ev = (self.csem[ek], self.ccnt[ek], self.ckey[ek])
        else:
            ev = (self.csem[ek], self.ccnt[ek] + 1, self.ckey[ek])
        self._commit(ev, ek, reads, writes, ())
        self.n_ins += 1
        return ev

    def dma(self, q, out, in_, reads=(), writes=(), accw=(), indirect=None, **kw):
        pool = self.dpool[q]
        slot = pool[self.dnext[q]]
        self.dnext[q] = (self.dnext[q] + 1) % len(pool)
        sem, cnt, key = slot
        self._wait(q, (sem, cnt, key))
        self._deps(q, reads, writes, accw)
        if indirect is None:
            ins = self.eng[q].dma_start(out=out, in_=in_, **kw)
        else:
            ins = self.eng[q].indirect_dma_start(out=out, in_=in_, **indirect)
        ins.then_inc(sem, 16)
        slot[1] = cnt + 16
        ev = (sem, cnt + 16, key)
        self._commit(ev, key, reads, writes, accw)
        self.n_ins += 1
        return ev

    def barrier(self):
        evs = [(self.csem[k], self.ccnt[k], self.ckey[k]) for k in self.csem if self.ccnt[k] > 0]
        for q in self.dpool:
            for sem, cnt, key in self.dpool[q]:
                if cnt > 0:
                    evs.append((sem, cnt, key))
        for ek in self.eng:
            for ev in evs:
                if ev[2] == self.ckey.get(ek):
                    continue
                self._wait(ek, ev)

    def finish(self):
        self.barrier()


class Rot:
    def __init__(self, items):
        self.items = items
        self.i = 0

    def next(self):
        it = self.items[self.i]
        self.i = (self.i + 1) % len(self.items)
        return it


def build_nc(n_layers=DEPTH, stop_phase=99):
    nc = bass.Bass("TRN2", target_bir_lowering=False)

    def din(name, shape, dt=F32):
        return nc.dram_tensor(name, list(shape), dt, kind="ExternalInput").ap()

    x_in = din("x", [4096, D])
    mem_in = din("mem", [256, D])
    w_in_a = din("w_in_a", [2, D, 5120])
    w_in_b = din("w_in_b", [2, D, 2560])
    sink_b = din("sink_b", [2, 12])
    w_mem_kv = din("w_mem_kv", [4, D, 1024])
    w_o = din("w_o", [4, D, D])
    ln1_g = din("ln1_g", [4, D])
    ln1_b = din("ln1_b", [4, D])
    router_w = din("router_w", [4, D, NE])
    router_b = din("router_b", [4, NE])
    w_gate_up = din("w_gate_up", [n_layers, NE, D, 2 * FE])
    b_gate_up = din("b_gate_up", [4, NE, 2 * FE])
    w_down = din("w_down", [n_layers, NE, FE, D])
    b_down = din("b_down", [4, NE, D])
    ln2_g = din("ln2_g", [4, D])
    ln2_b = din("ln2_b", [4, D])
    consts = din("consts", [128, 128 * 3 + 32])
    biasA = din("biasA", [128, 12, 256])
    biasB = din("biasB", [128, 12, 384])
    out = nc.dram_tensor("out", [4096, D], F32, kind="ExternalOutput").ap()

    def dscr(name, shape, dt):
        return nc.dram_tensor(name, list(shape), dt).ap()

    XS = dscr("XS", [4096, D], F32)
    QT = dscr("QT", [16, 128, 4096], BF16)
    KT = dscr("KT", [12, 128, 4096], BF16)
    VV = dscr("VV", [4096, 1536], BF16)
    HEADS = dscr("HEADS", [4096, D], BF16)
    LSE = dscr("LSE", [4096, 12], F32)
    HH = dscr("HH", [4096, D], F32)
    XG = dscr("XG", [NE * 640, D], BF16)
    YG = dscr("YG", [NE * 640, D], F32)

    b_XS = [Buf() for _ in range(32)]
    b_QT = [Buf() for _ in range(16)]
    b_KT = [Buf() for _ in range(12)]
    b_VV = [Buf() for _ in range(32)]
    b_HEADS = [Buf() for _ in range(MAXT)]
    b_LSE = [Buf() for _ in range(MAXT)]
    b_HH = [Buf() for _ in range(MAXT)]
    b_XG = [Buf() for _ in range(NE)]
    b_YG = [Buf() for _ in range(NE)]
    b_OUT = Buf()

    with contextlib.ExitStack() as st:
        s = Sched(nc, st)

        uniq = [0]

        def sbt(stack, name, shape, dt):
            uniq[0] += 1
            return stack.enter_context(nc.sbuf_tensor("%s_%d" % (name, uniq[0]), list(shape), dt))

        cst = sbt(st, "cst", [128, 416], F32); b_cst = Buf()
        identb = sbt(st, "identb", [128, 128], BF16); b_identb = Buf()
        memT = sbt(st, "memT", [128, KC, 256], BF16); b_memT = Buf()
        MKT = sbt(st, "MKT", [128, 4, 256], BF16); b_MKT = Buf()
        MV = sbt(st, "MV", [128, 2, 512], BF16); b_MV = Buf()
        GATES = sbt(st, "GATES", [128, MAXT * 4], F32)
        DEST = sbt(st, "DEST", [128, MAXT * 4], I32)
        b_route = [Buf() for _ in range(MAXT)]
        BASE = sbt(st, "BASE", [128, NE], F32); b_BASE = Buf()
        identf = cst[:, 0:128]
        ltri = cst[:, 128:256]
        onesf = cst[:, 256:384]
        iota32 = cst[:, 384:416]
        PF = [st.enter_context(nc.psum_tensor("PF%d" % i, [128, 512], F32)) for i in range(6)]
        b_PF = [Buf() for _ in range(6)]
        PT = [st.enter_context(nc.psum_tensor("PT%d" % i, [128, 8, 128], BF16)) for i in range(2)]
        b_PT = [Buf() for _ in range(2)]
        pf_rot = Rot(list(zip(PF, b_PF)))
        pt_rot = Rot(list(zip(PT, b_PT)))
        ev_rot = Rot(["act", "dve"])

        bound_regs = {}
        for cp in sorted(set(CAP)):
            rg = nc.gpsimd.alloc_register("bnd%d" % cp)
            nc.gpsimd.reg_mov(rg, NE * cp - 1)
            bound_regs[cp] = rg
        s.dma("sp", cst[:], consts, writes=[b_cst])
        s.op("dve", lambda e: e.tensor_copy(out=identb[:], in_=identf), reads=[b_cst], writes=[b_identb])

        def transpose_bf(dst_fn, src_fn, n, reads, writes_fn):
            i = 0
            while i < n:
                cnt = min(8, n - i)
                pt, bpt = pt_rot.next()
                for j in range(cnt):
                    s.op("pe", lambda e, j=j: e.transpose(out=pt[:, j, :], in_=src_fn(i + j), identity=identb[:]),
                         reads=list(reads) + [b_identb], writes=[bpt])
                ek = ev_rot.next()
                if ek == "act":
                    s.op("act", lambda e: e.copy(out=dst_fn(i, cnt), in_=pt[:, 0:cnt, :]), reads=[bpt], writes=writes_fn(i, cnt))
                else:
                    s.op("dve", lambda e: e.tensor_copy(out=dst_fn(i, cnt), in_=pt[:, 0:cnt, :]), reads=[bpt], writes=writes_fn(i, cnt))
                i += cnt

        with contextlib.ExitStack() as ph:
            mf = sbt(ph, "mf", [128, 2, D], F32); b_mf = Buf()
            mb = sbt(ph, "mb", [128, 2, D], BF16); b_mb = Buf()
            s.dma("sp", mf[:], mem_in.rearrange("(j p) n -> p j n", p=128), writes=[b_mf])
            s.op("dve", lambda e: e.tensor_copy(out=mb[:], in_=mf[:]), reads=[b_mf], writes=[b_mb])
            for j in range(2):
                transpose_bf(lambda i0, cnt, j=j: memT[:, i0:i0 + cnt, j * 128:(j + 1) * 128],
                             lambda i, j=j: mb[:, j, i * 128:(i + 1) * 128], KC, [b_mb], lambda i0, cnt: [b_memT])
            s.barrier()

        def layer_norm(stack_tiles, y, b_y, o, b_o, Gbc, Bbc, b_gb):
            stats, b_stats, mv, b_mv, rstd, b_rstd, nmr, b_nmr = stack_tiles
            for c in range(4):
                s.op("dve", lambda e, c=c: e.bn_stats(out=stats[:, c, :], in_=y[:, c * 512:(c + 1) * 512]),
                     reads=[b_y], writes=[b_stats])
            s.op("dve", lambda e: e.bn_aggr(out=mv[:], in_=stats[:].rearrange("p a b -> p (a b)")), reads=[b_stats], writes=[b_mv])
            s.op("dve", lambda e: e.tensor_scalar(out=rstd[:], in0=mv[:, 1:2], scalar1=EPS, scalar2=None,
                                                  op0=ALU.add), reads=[b_mv], writes=[b_rstd])
            s.op("act", lambda e: e.activation(out=rstd[:], in_=rstd[:], func=AF.Ln), reads=[b_rstd], writes=[b_rstd])
            s.op("act", lambda e: e.activation(out=rstd[:], in_=rstd[:], func=AF.Exp, scale=-0.5), reads=[b_rstd], writes=[b_rstd])
            s.op("dve", lambda e: e.scalar_tensor_tensor(out=nmr[:], in0=mv[:, 0:1], scalar=-1.0, in1=rstd[:],
                                                         op0=ALU.mult, op1=ALU.mult), reads=[b_mv, b_rstd], writes=[b_nmr])
            s.op("act", lambda e: e.activation(out=o[:], in_=y[:], func=AF.Identity, bias=nmr[:], scale=rstd[:]),
                 reads=[b_y, b_rstd, b_nmr], writes=[b_o])
            s.op("pool", lambda e: e.tensor_tensor(out=o[:], in0=o[:], in1=Gbc[:], op=ALU.mult), reads=[b_o, b_gb], writes=[b_o])
            s.op("dve", lambda e: e.tensor_tensor(out=o[:], in0=o[:], in1=Bbc[:], op=ALU.add), reads=[b_o, b_gb], writes=[b_o])

        for l in range(n_layers):
            if l > 0:
                s.new_epoch("e%d" % l)
            is_a = (l % 2 == 0)
            lj = l // 2
            nq, nk, cap = NQ[l], NK[l], CAP[l]
            nqt, nkt = nq // 128, nk // 128
            w_in = w_in_a[lj] if is_a else w_in_b[lj]
            ncols = 5120 if is_a else 2560
            x_src = x_in if l == 0 else XS

            def unit_kind(u):
                if u < 12:
                    return ("q", u)
                if is_a:
                    if u < 24:
                        return ("k", u - 12)
                    if u < 36:
                        return ("v", u - 24)
                    return ("q", 12 + (u - 36))
                if u < 14:
                    return ("k", u - 12)
                if u < 16:
                    return ("v", u - 14)
                return ("q", 12 + (u - 16))

            with contextlib.ExitStack() as ph:
                wm = sbt(ph, "wm", [128, KC, 512], BF16); b_wm = Buf()
                XT = sbt(ph, "XT", [128, KC, 2048], BF16); b_XT = [Buf() for _ in range(16)]
                wg = [sbt(ph, "wg%d" % i, [128, KC, 512], BF16) for i in range(2)]
                b_wg = [Buf() for _ in range(2)]
                wg_rot = Rot(list(zip(wg, b_wg)))
                xf = [sbt(ph, "xf%d" % i, [128, D], F32) for i in range(2)]; b_xf = [Buf(), Buf()]
                xb = [sbt(ph, "xb%d" % i, [128, D], BF16) for i in range(2)]; b_xb = [Buf(), Buf()]
                xf_rot = Rot(list(zip(xf, b_xf, xb, b_xb)))
                ost = [sbt(ph, "ost%d" % i, [128, 512], BF16) for i in range(4)]; b_ost = [Buf() for _ in range(4)]
                ost_rot = Rot(list(zip(ost, b_ost)))

                for half in range(2):
                    s.dma("pool", wm[:], w_mem_kv[l, :, half * 512:(half + 1) * 512].rearrange("(k p) n -> p k n", p=128),
                          writes=[b_wm])
                    if half == 0:
                        for h in range(4):
                            pf, bpf = pf_rot.next()
                            for k in range(KC):
                                s.op("pe", lambda e, k=k, h=h: e.matmul(pf[:, 0:256], lhsT=wm[:, k, h * 128:(h + 1) * 128],
                                                                        rhs=memT[:, k, :], start=(k == 0), stop=(k == KC - 1)),
                                     reads=[b_wm, b_memT], writes=[bpf], inc=(k == KC - 1))
                            s.op("act", lambda e, h=h: e.copy(out=MKT[:, h, :], in_=pf[:, 0:256]), reads=[bpf], writes=[b_MKT])
                    else:
                        for j in range(2):
                            pf, bpf = pf_rot.next()
                            for k in range(KC):
                                s.op("pe", lambda e, k=k, j=j: e.matmul(pf[:], lhsT=memT[:, k, j * 128:(j + 1) * 128],
                                                                        rhs=wm[:, k, :], start=(k == 0), stop=(k == KC - 1)),
                                     reads=[b_wm, b_memT], writes=[bpf], inc=(k == KC - 1))
                            s.op("act", lambda e, j=j: e.copy(out=MV[:, j, :], in_=pf[:]), reads=[bpf], writes=[b_MV])

                for t0 in range(0, nk, 2048):
                    t1 = min(t0 + 2048, nk)
                    ntl = (t1 - t0) // 128
                    for ti in range(ntl):
                        gt = t0 // 128 + ti
                        xft, bxf, xbt, bxb = xf_rot.next()
                        s.dma("sp", xft[:], x_src[gt * 128:(gt + 1) * 128, :], reads=[b_XS[gt]], writes=[bxf])
                        s.op("pool", lambda e: e.tensor_copy(out=xbt[:], in_=xft[:]), reads=[bxf], writes=[bxb])
                        transpose_bf(lambda i0, cnt, ti=ti: XT[:, i0:i0 + cnt, ti * 128:(ti + 1) * 128],
                                     lambda i, xbt=xbt: xbt[:, i * 128:(i + 1) * 128], KC, [bxb],
                                     lambda i0, cnt, ti=ti: [b_XT[ti]])
                    for cg in range(ncols // 512):
                        wgt, bwg = wg_rot.next()
                        s.dma("pool", wgt[:], w_in[:, cg * 512:(cg + 1) * 512].rearrange("(k p) n -> p k n", p=128),
                              writes=[bwg])
                        kinds = [unit_kind(cg * 4 + j) for j in range(4)]
                        for j, (kd, hidx) in enumerate(kinds):
                            if kd == "v":
                                continue
                            lim = nq if kd == "q" else nk
                            dst = QT if kd == "q" else KT
                            bdst = b_QT[hidx] if kd == "q" else b_KT[hidx]
                            for c0 in range(t0, min(t1, lim), 512):
                                c1 = min(c0 + 512, t1, lim)
                                n = c1 - c0
                                pf, bpf = pf_rot.next()
                                tl = [b_XT[(c0 - t0) // 128 + i] for i in range((n + 127) // 128)]
                                for k in range(KC):
                                    s.op("pe", lambda e, k=k, j=j, c0=c0, n=n: e.matmul(
                                        pf[:, 0:n], lhsT=wgt[:, k, j * 128:(j + 1) * 128],
                                        rhs=XT[:, k, c0 - t0:c0 - t0 + n], start=(k == 0), stop=(k == KC - 1)),
                                        reads=[bwg] + tl, writes=[bpf], inc=(k == KC - 1))
                                o_t, b_o = ost_rot.next()
                                ek = ev_rot.next()
                                if ek == "act":
                                    s.op("act", lambda e, n=n: e.copy(out=o_t[:, 0:n], in_=pf[:, 0:n]), reads=[bpf], writes=[b_o])
                                else:
                                    s.op("dve", lambda e, n=n: e.tensor_copy(out=o_t[:, 0:n], in_=pf[:, 0:n]), reads=[bpf], writes=[b_o])
                                s.dma("sp", dst[hidx, :, c0:c1], o_t[:, 0:n], reads=[b_o], accw=[bdst])
                        vj = [j for j, (kd, _) in enumerate(kinds) if kd == "v"]
                        if vj:
                            j0 = vj[0]
                            nv = len(vj) * 128
                            vcol = kinds[j0][1] * 128
                            for ti in range(ntl):
                                gt = t0 // 128 + ti
                                pf, bpf = pf_rot.next()
                                for k in range(KC):
                                    s.op("pe", lambda e, k=k, ti=ti: e.matmul(
                                        pf[:, 0:nv], lhsT=XT[:, k, ti * 128:(ti + 1) * 128],
                                        rhs=wgt[:, k, j0 * 128:j0 * 128 + nv], start=(k == 0), stop=(k == KC - 1)),
                                        reads=[bwg, b_XT[ti]], writes=[bpf], inc=(k == KC - 1))
                                o_t, b_o = ost_rot.next()
                                ek = ev_rot.next()
                                if ek == "act":
                                    s.op("act", lambda e: e.copy(out=o_t[:, 0:nv], in_=pf[:, 0:nv]), reads=[bpf], writes=[b_o])
                                else:
                                    s.op("dve", lambda e: e.tensor_copy(out=o_t[:, 0:nv], in_=pf[:, 0:nv]), reads=[bpf], writes=[b_o])
                                s.dma("sp", VV[gt * 128:(gt + 1) * 128, vcol:vcol + nv], o_t[:, 0:nv], reads=[b_o], accw=[b_VV[gt]])
                s.barrier()

            if stop_phase < 2:
                break
            with contextlib.ExitStack() as ph:
                NB = 3
                sc_t = [sbt(ph, "sc%d" % i, [128, 384], F32) for i in range(NB)]; b_sc = [Buf() for _ in range(NB)]
                p_t = [sbt(ph, "p%d" % i, [128, 384], BF16) for i in range(NB)]; b_p = [Buf() for _ in range(NB)]
                pT_t = [sbt(ph, "pT%d" % i, [128, 3, 128], BF16) for i in range(NB)]; b_pT = [Buf() for _ in range(NB)]
                sm_t = [sbt(ph, "sm%d" % i, [128, 8], F32) for i in range(NB)]; b_sm = [Buf() for _ in range(NB)]
                blk_rot = Rot(list(zip(sc_t, b_sc, p_t, b_p, pT_t, b_pT, sm_t, b_sm)))
                for i in range(NB):
                    s.op("pool", lambda e, i=i: e.memset(p_t[i][:], 0.0), writes=[b_p[i]])

                def attn_block(qT, kT, rq, rk, bias, rb, c_lo, c_hi, chunks, sink, o_dst, b_o, lse_dst, b_lse):
                    sc, bsc, p, bp, pT, bpT, sm, bsm = blk_rot.next()
                    n = c_hi - c_lo
                    pf, bpf = pf_rot.next()
                    s.op("pe", lambda e: e.matmul(pf[:, c_lo:c_hi], lhsT=qT, rhs=kT, start=True, stop=True),
                         reads=[rq, rk], writes=[bpf])
                    if bias is not None:
                        s.op("dve", lambda e: e.scalar_tensor_tensor(out=sc[:, c_lo:c_hi], in0=pf[:, c_lo:c_hi], scalar=SCALE,
                                                                     in1=bias, op0=ALU.mult, op1=ALU.add),
                             reads=[bpf, rb], writes=[bsc])
                        s.op("dve", lambda e: e.reduce_max(out=sm[:, 0:1], in_=sc[:, c_lo:c_hi], axis=AX.X),
                             reads=[bsc], writes=[bsm])
                        if sink is not None:
                            sk_ap, b_sk = sink
                            s.op("dve", lambda e: e.tensor_tensor(out=sm[:, 0:1], in0=sm[:, 0:1], in1=sk_ap, op=ALU.max),
                                 reads=[bsm, b_sk], writes=[bsm])
                        s.op("dve", lambda e: e.tensor_scalar(out=sm[:, 1:2], in0=sm[:, 0:1], scalar1=-1.0, scalar2=None,
                                                              op0=ALU.mult), reads=[bsm], writes=[bsm])
                        s.op("act", lambda e: e.activation(out=p[:, c_lo:c_hi], in_=sc[:, c_lo:c_hi], func=AF.Exp,
                                                           bias=sm[:, 1:2], scale=1.0, accum_out=sm[:, 2:3]),
                             reads=[bsc, bsm], writes=[bp, bsm])
                    else:
                        s.op("dve", lambda e: e.reduce_max(out=sm[:, 0:1], in_=pf[:, c_lo:c_hi], axis=AX.X),
                             reads=[bpf], writes=[bsm])
                        s.op("dve", lambda e: e.tensor_scalar(out=sm[:, 1:2], in0=sm[:, 0:1], scalar1=-SCALE, scalar2=None,
                                                              op0=ALU.mult), reads=[bsm], writes=[bsm])
                        s.op("act", lambda e: e.activation(out=p[:, c_lo:c_hi], in_=pf[:, c_lo:c_hi], func=AF.Exp,
                                                           bias=sm[:, 1:2], scale=SCALE, accum_out=sm[:, 2:3]),
                             reads=[bpf, bsm], writes=[bp, bsm])
                    if sink is not None:
                        sk_ap, b_sk = sink
                        s.op("act", lambda e: e.activation(out=sm[:, 3:4], in_=sk_ap, func=AF.Exp, bias=sm[:, 1:2], scale=1.0),
                             reads=[bsm, b_sk], writes=[bsm])
                        s.op("dve", lambda e: e.tensor_tensor(out=sm[:, 2:3], in0=sm[:, 2:3], in1=sm[:, 3:4], op=ALU.add),
                             reads=[bsm], writes=[bsm])
                    s.op("dve", lambda e: e.reciprocal(out=sm[:, 4:5], in_=sm[:, 2:3]), reads=[bsm], writes=[bsm])
                    if lse_dst is not None:
                        s.op("act", lambda e: e.activation(out=sm[:, 5:6], in_=sm[:, 2:3], func=AF.Ln), reads=[bsm], writes=[bsm])
                        s.op("dve", lambda e: e.tensor_tensor(out=lse_dst, in0=sm[:, 5:6], in1=sm[:, 0:1], op=ALU.add),
                             reads=[bsm], writes=[b_lse])
                    pt, bpt = pt_rot.next()
                    for idx, (ci, rhs, rbufs) in enumerate(chunks):
                        s.op("pe", lambda e, idx=idx, ci=ci: e.transpose(out=pt[:, idx, :], in_=p[:, ci * 128:(ci + 1) * 128],
                                                                         identity=identb[:]),
                             reads=[bp, b_identb], writes=[bpt])
                    nch = len(chunks)
                    s.op("act", lambda e: e.copy(out=pT[:, 0:nch, :], in_=pt[:, 0:nch, :]), reads=[bpt], writes=[bpT])
                    pf2, bpf2 = pf_rot.next()
                    for idx, (ci, rhs, rbufs) in enumerate(chunks):
                        s.op("pe", lambda e, idx=idx, rhs=rhs: e.matmul(pf2[:, 0:128], lhsT=pT[:, idx, :], rhs=rhs,
                                                                        start=(idx == 0), stop=(idx == nch - 1)),
                             reads=[bpT] + list(rbufs), writes=[bpf2], inc=(idx == nch - 1))
                    s.op("dve", lambda e: e.tensor_scalar(out=o_dst, in0=pf2[:, 0:128], scalar1=sm[:, 4:5], scalar2=None,
                                                          op0=ALU.mult), reads=[bpf2, bsm], writes=[b_o])

                def fix_p_zero(c_lo, c_hi, width):
                    for i in range(NB):
                        if c_lo > 0:
                            s.op("pool", lambda e, i=i: e.memset(p_t[i][:, 0:c_lo], 0.0), writes=[b_p[i]])
                        if c_hi < width:
                            s.op("pool", lambda e, i=i: e.memset(p_t[i][:, c_hi:width], 0.0), writes=[b_p[i]])

                with contextlib.ExitStack() as ph2:
                    qm = sbt(ph2, "qm", [128, 4, nq], BF16); b_qm = Buf()
                    for h in range(4):
                        s.dma("sp", qm[:, h, :], QT[12 + h, :, 0:nq], reads=[b_QT[12 + h]], writes=[b_qm])
                    ost2 = [sbt(ph2, "mo%d" % i, [128, 512], BF16) for i in range(2)]; b_ost2 = [Buf(), Buf()]
                    o_rot = Rot(list(zip(ost2, b_ost2)))
                    for blk in range(nqt):
                        o_t, b_o = o_rot.next()
                        for h in range(4):
                            chunks = [(ci, MV[:, ci, h * 128:(h + 1) * 128], [b_MV]) for ci in range(2)]
                            attn_block(qm[:, h, blk * 128:(blk + 1) * 128], MKT[:, h, :], b_qm, b_MKT, None, None,
                                       0, 256, chunks, None, o_t[:, h * 128:(h + 1) * 128], b_o, None, None)
                        s.dma("sp", HEADS[blk * 128:(blk + 1) * 128, 1536:2048], o_t[:], reads=[b_o], accw=[b_HEADS[blk]])
                    s.barrier()

                if is_a:
                    for g in range(3):
                        d = DIL[g]
                        lq, lk = nq // d, nk // d
                        with contextlib.ExitStack() as ph2:
                            bt = sbt(ph2, "bt", [128, 4, 256], F32); b_bt = Buf()
                            s.dma("sp", bt[:], biasA[:, 4 * g:4 * g + 4, :], writes=[b_bt])
                            kt = sbt(ph2, "kt", [128, 4, nk], BF16); b_kt = Buf()
                            qt = sbt(ph2, "qt", [128, 4, nq], BF16); b_qt = Buf()
                            for hh in range(4):
                                s.dma("sp", kt[:, hh, :], KT[4 * g + hh, :, 0:nk], reads=[b_KT[4 * g + hh]], writes=[b_kt])
                                s.dma("sp", qt[:, hh, :], QT[4 * g + hh, :, 0:nq], reads=[b_QT[4 * g + hh]], writes=[b_qt])
                            vch = [sbt(ph2, "vch%d" % i, [128, 512], BF16) for i in range(6)]; b_vch = [Buf() for _ in range(6)]
                            for i in range(6):
                                s.op("pool", lambda e, i=i: e.memset(vch[i][:], 0.0), writes=[b_vch[i]])
                            v_rot = Rot(list(zip(vch, b_vch)))
                            ost2 = [sbt(ph2, "ao%d" % i, [128, 512], BF16) for i in range(2)]; b_ost2 = [Buf(), Buf()]
                            lst2 = [sbt(ph2, "al%d" % i, [128, 4], F32) for i in range(2)]; b_lst2 = [Buf(), Buf()]
                            o_rot = Rot(list(zip(ost2, b_ost2, lst2, b_lst2)))
                            starts = list(range(0, lq - 127, 128))
                            if lq % 128:
                                starts.append(lq - 128)
                            p_state = (0, 256)
                            for r in range(d):
                                for i0 in starts:
                                    j0 = i0 - 64
                                    c_lo = max(0, -j0)
                                    c_hi = min(256, lk - j0)
                                    if (c_lo, c_hi) != p_state:
                                        fix_p_zero(c_lo, c_hi, 256)
                                        p_state = (c_lo, c_hi)
                                    chunks_v = []
                                    for ci in range(2):
                                        jb = j0 + 128 * ci
                                        p_lo = max(0, -jb)
                                        p_hi = min(128, lk - jb)
                                        if p_hi <= p_lo:
                                            continue
                                        vt, bv = v_rot.next()
                                        tok0 = r + d * (jb + p_lo)
                                        cnt = p_hi - p_lo
                                        tok_last = tok0 + d * (cnt - 1)
                                        tiles = sorted(set(range(tok0 // 128, tok_last // 128 + 1)))
                                        s.dma("act", vt[p_lo:p_hi, :], VV[tok0:tok_last + 1:d, 512 * g:512 * g + 512],
                                              reads=[b_VV[t] for t in tiles], writes=[bv])
                                        chunks_v.append((ci, vt, bv))
                                    o_t, b_o, l_t, b_l = o_rot.next()
                                    q0 = r + d * i0
                                    qlast = q0 + d * 127
                                    k0 = r + d * (j0 + c_lo)
                                    klast = r + d * (j0 + c_hi - 1)
                                    for hh in range(4):
                                        chunks = [(ci, vt[:, hh * 128:(hh + 1) * 128], [bv]) for (ci, vt, bv) in chunks_v]
                                        attn_block(qt[:, hh, q0:qlast + 1:d], kt[:, hh, k0:klast + 1:d], b_qt, b_kt,
                                                   bt[:, hh, c_lo:c_hi], b_bt, c_lo, c_hi, chunks, None,
                                                   o_t[:, hh * 128:(hh + 1) * 128], b_o, l_t[:, hh:hh + 1], b_l)
                                    tiles = sorted(set(range(q0 // 128, qlast // 128 + 1)))
                                    s.dma("sp", HEADS[q0:qlast + 1:d, 512 * g:512 * g + 512], o_t[:], reads=[b_o],
                                          accw=[b_HEADS[t] for t in tiles])
                                    s.dma("sp", LSE[q0:qlast + 1:d, 4 * g:4 * g + 4], l_t[:], reads=[b_l],
                                          accw=[b_LSE[t] for t in tiles])
                            if p_state != (0, 256):
                                fix_p_zero(0, 256, 256)
                            s.barrier()
                else:
                    with contextlib.ExitStack() as ph2:
                        bt = sbt(ph2, "btb", [128, 12, 384], F32); b_bt = Buf()
                        s.dma("sp", bt[:], biasB, writes=[b_bt])
                        sk = sbt(ph2, "sk", [128, 12], F32); b_sk = Buf()
                        s.dma("sp", sk[:], sink_b[lj:lj + 1, :].broadcast_to([128, 12]), writes=[b_sk])
                        kt = sbt(ph2, "ktb", [128, 2, nk], BF16); b_kt = Buf()
                        for kv in range(2):
                            s.dma("sp", kt[:, kv, :], KT[kv, :, 0:nk], reads=[b_KT[kv]], writes=[b_kt])
                        vs = sbt(ph2, "vsb", [128, nkt, 256], BF16); b_vs = Buf()
                        s.dma("sp", vs[:], VV[0:nk, 0:256].rearrange("(j p) n -> p j n", p=128),
                              reads=[b_VV[t] for t in range(nkt)], writes=[b_vs])
                        qt = sbt(ph2, "qtb", [128, 6, nq], BF16); b_qt = Buf()
                        ost2 = [sbt(ph2, "bo%d" % i, [128, 768], BF16) for i in range(2)]; b_ost2 = [Buf(), Buf()]
                        o_rot = Rot(list(zip(ost2, b_ost2)))
                        pb_state = [(0, 384)]
                        for kv in range(2):
                            for hh in range(6):
                                s.dma("sp", qt[:, hh, :], QT[6 * kv + hh, :, 0:nq], reads=[b_QT[6 * kv + hh]], writes=[b_qt])
                            for blk in range(nqt):
                                i0 = blk * 128
                                c_lo = 128 if blk == 0 else 0
                                c_hi = min(384, nk - i0 + 128)
                                if (c_lo, c_hi) != pb_state[0]:
                                    fix_p_zero(c_lo, c_hi, 384)
                                    pb_state[0] = (c_lo, c_hi)
                                o_t, b_o = o_rot.next()
                                for hh in range(6):
                                    h = 6 * kv + hh
                                    chunks = [(ci, vs[:, blk - 1 + ci, kv * 128:(kv + 1) * 128], [b_vs])
                                              for ci in range(3) if 0 <= blk - 1 + ci < nkt]
                                    attn_block(qt[:, hh, i0:i0 + 128], kt[:, kv, i0 - 128 + c_lo:i0 - 128 + c_hi], b_qt, b_kt,
                                               bt[:, h, c_lo:c_hi], b_bt, c_lo, c_hi, chunks, (sk[:, h:h + 1], b_sk),
                                               o_t[:, hh * 128:(hh + 1) * 128], b_o, None, None)
                                s.dma("sp", HEADS[i0:i0 + 128, 768 * kv:768 * kv + 768], o_t[:], reads=[b_o],
                                      accw=[b_HEADS[blk]])
                        s.barrier()

            if stop_phase < 3:
                break
            with contextlib.ExitStack() as ph:
                wo = sbt(ph, "wo", [128, KC, D], BF16); b_wo = Buf()
                for c in range(4):
                    s.dma("pool", wo[:, :, c * 512:(c + 1) * 512],
                          w_o[l, :, c * 512:(c + 1) * 512].rearrange("(k p) n -> p k n", p=128), accw=[b_wo])
                G1 = sbt(ph, "G1", [128, D], F32); B1 = sbt(ph, "B1", [128, D], F32); b_gb = Buf()
                s.dma("sp", G1[:], ln1_g[l:l + 1, :].broadcast_to([128, D]), accw=[b_gb])
                s.dma("sp", B1[:], ln1_b[l:l + 1, :].broadcast_to([128, D]), accw=[b_gb])
                rw = sbt(ph, "rw", [128, KC, NE], F32); b_rw = Buf()
                s.dma("sp", rw[:], router_w[l].rearrange("(k p) n -> p k n", p=128), writes=[b_rw])
                rb = sbt(ph, "rb", [128, NE], F32); b_rb = Buf()
                s.dma("sp", rb[:], router_b[l:l + 1, :].broadcast_to([128, NE]), writes=[b_rb])
                s.op("pool", lambda e: e.memset(BASE[:], 0.0), writes=[b_BASE])
                NB3 = 2
                hd = [sbt(ph, "hd%d" % i, [128, D], BF16) for i in range(NB3)]; b_hd = [Buf() for _ in range(NB3)]
                ls = [sbt(ph, "ls%d" % i, [128, 12], F32) for i in range(NB3)]; b_ls = [Buf() for _ in range(NB3)]
                mw = [sbt(ph, "mw%d" % i, [128, 24], F32) for i in range(NB3)]; b_mw = [Buf() for _ in range(NB3)]
                hT = [sbt(ph, "hT%d" % i, [128, KC, 128], BF16) for i in range(NB3)]; b_hT = [Buf() for _ in range(NB3)]
                xr = [sbt(ph, "xr%d" % i, [128, D], F32) for i in range(NB3)]; b_xr = [Buf() for _ in range(NB3)]
                yy = [sbt(ph, "yy%d" % i, [128, D], F32) for i in range(NB3)]; b_yy = [Buf() for _ in range(NB3)]
                hh_t = [sbt(ph, "hh%d" % i, [128, D], F32) for i in range(NB3)]; b_hh = [Buf() for _ in range(NB3)]
                hb = [sbt(ph, "hb%d" % i, [128, D], BF16) for i in range(NB3)]; b_hb = [Buf() for _ in range(NB3)]
                hTf = [sbt(ph, "hTf%d" % i, [128, KC, 128], F32) for i in range(1)]; b_hTf = [Buf()]
                lnt = [(sbt(ph, "st%d" % i, [128, 4, 6], F32), Buf(), sbt(ph, "mv%d" % i, [128, 2], F32), Buf(),
                        sbt(ph, "rs%d" % i, [128, 1], F32), Buf(), sbt(ph, "nm%d" % i, [128, 1], F32), Buf())
                       for i in range(NB3)]
                rt = [sbt(ph, "rt%d" % i, [128, 256], F32) for i in range(NB3)]; b_rt = [Buf() for _ in range(NB3)]
                bound = bound_regs[cap]
                for t in range(nqt):
                    i = t % NB3
                    s.dma("sp", hd[i][:], HEADS[t * 128:(t + 1) * 128, :], reads=[b_HEADS[t]], writes=[b_hd[i]])
                    s.dma("sp", xr[i][:], x_src[t * 128:(t + 1) * 128, :], reads=[b_XS[t]], writes=[b_xr[i]])
                    if is_a:
                        s.dma("sp", ls[i][:], LSE[t * 128:(t + 1) * 128, :], reads=[b_LSE[t]], writes=[b_ls[i]])
                        m = mw[i]
                        bm = b_mw[i]
                        s.op("dve", lambda e: e.tensor_tensor(out=m[:, 0:4], in0=ls[i][:, 0:4], in1=ls[i][:, 4:8], op=ALU.max),
                             reads=[b_ls[i]], writes=[bm])
                        s.op("dve", lambda e: e.tensor_tensor(out=m[:, 0:4], in0=m[:, 0:4], in1=ls[i][:, 8:12], op=ALU.max),
                             reads=[b_ls[i], bm], writes=[bm])
                        for g in range(3):
                            s.op("dve", lambda e, g=g: e.tensor_tensor(out=m[:, 4 + 4 * g:8 + 4 * g], in0=ls[i][:, 4 * g:4 * g + 4],
                                                                       in1=m[:, 0:4], op=ALU.subtract),
                                 reads=[b_ls[i], bm], writes=[bm])
                        s.op("act", lambda e: e.activation(out=m[:, 4:16], in_=m[:, 4:16], func=AF.Exp), reads=[bm], writes=[bm])
                        s.op("dve", lambda e: e.tensor_tensor(out=m[:, 16:20], in0=m[:, 4:8], in1=m[:, 8:12], op=ALU.add),
                             reads=[bm], writes=[bm])
                        s.op("dve", lambda e: e.tensor_tensor(out=m[:, 16:20], in0=m[:, 16:20], in1=m[:, 12:16], op=ALU.add),
                             reads=[bm], writes=[bm])
                        s.op("dve", lambda e: e.reciprocal(out=m[:, 20:24], in_=m[:, 16:20]), reads=[bm], writes=[bm])
                        for g in range(3):
                            s.op("dve", lambda e, g=g: e.tensor_tensor(out=m[:, 4 + 4 * g:8 + 4 * g], in0=m[:, 4 + 4 * g:8 + 4 * g],
                                                                       in1=m[:, 20:24], op=ALU.mult), reads=[bm], writes=[bm])
                        for hx in range(12):
                            ek = "pool" if hx % 2 else "dve"
                            s.op(ek, lambda e, hx=hx: e.tensor_scalar(out=hd[i][:, hx * 128:(hx + 1) * 128],
                                                                      in0=hd[i][:, hx * 128:(hx + 1) * 128],
                                                                      scalar1=m[:, 4 + hx:5 + hx], scalar2=None, op0=ALU.mult),
                                 reads=[b_hd[i], bm], writes=[b_hd[i]])
                    transpose_bf(lambda i0, cnt: hT[i][:, i0:i0 + cnt, :], lambda k: hd[i][:, k * 128:(k + 1) * 128], KC,
                                 [b_hd[i]], lambda i0, cnt: [b_hT[i]])
                    pfs = [pf_rot.next() for _ in range(4)]
                    for c in range(4):
                        pf, bpf = pfs[c]
                        for k in range(KC):
                            s.op("pe", lambda e, k=k, c=c, pf=pf: e.matmul(pf[:], lhsT=hT[i][:, k, :], rhs=wo[:, k, c * 512:(c + 1) * 512],
                                                                           start=(k == 0), stop=(k == KC - 1)),
                                 reads=[b_hT[i], b_wo], writes=[bpf], inc=(k == KC - 1))
                    for c in range(4):
                        pf, bpf = pfs[c]
                        s.op("dve", lambda e, c=c, pf=pf: e.scalar_tensor_tensor(out=yy[i][:, c * 512:(c + 1) * 512],
                                                                                 in0=xr[i][:, c * 512:(c + 1) * 512], scalar=ALPHA,
                                                                                 in1=pf[:], op0=ALU.mult, op1=ALU.add),
                             reads=[b_xr[i], bpf], writes=[b_yy[i]])
                    layer_norm(lnt[i], yy[i], b_yy[i], hh_t[i], b_hh[i], G1, B1, b_gb)
                    s.dma("sp", HH[t * 128:(t + 1) * 128, :], hh_t[i][:], reads=[b_hh[i]], writes=[b_HH[t]])
                    s.op("act", lambda e: e.copy(out=hb[i][:], in_=hh_t[i][:]), reads=[b_hh[i]], writes=[b_hb[i]])
                    for k4 in range(4):
                        pf, bpf = pf_rot.next()
                        for j in range(4):
                            k = k4 * 4 + j
                            s.op("pe", lambda e, k=k, j=j, pf=pf: e.transpose(out=pf[:, j * 128:(j + 1) * 128],
                                                                              in_=hh_t[i][:, k * 128:(k + 1) * 128], identity=identf),
                                 reads=[b_hh[i], b_cst], writes=[bpf])
                        s.op("act", lambda e, k4=k4, pf=pf: e.copy(out=hTf[0][:, k4 * 4:(k4 + 1) * 4, :],
                                                                   in_=pf[:].rearrange("p (a b) -> p a b", a=4)),
                             reads=[bpf], writes=[b_hTf[0]])
                    pf, bpf = pf_rot.next()
                    for k in range(KC):
                        s.op("pe", lambda e, k=k, pf=pf: e.matmul(pf[:, 0:NE], lhsT=hTf[0][:, k, :], rhs=rw[:, k, :],
                                                                  start=(k == 0), stop=(k == KC - 1)),
                             reads=[b_hTf[0], b_rw], writes=[bpf], inc=(k == KC - 1))
                    r_ = rt[i]
                    br = b_rt[i]
                    s.op("dve", lambda e, pf=pf: e.tensor_tensor(out=r_[:, 0:32], in0=pf[:, 0:NE], in1=rb[:], op=ALU.add),
                         reads=[bpf, b_rb], writes=[br])
                    s.op("dve", lambda e: e.max(out=r_[:, 32:40], in_=r_[:, 0:32]), reads=[br], writes=[br])
                    s.op("dve", lambda e: e.max_index(out=r_[:, 40:48].bitcast(U32), in_max=r_[:, 32:40], in_values=r_[:, 0:32]),
                         reads=[br], writes=[br])
                    s.op("dve", lambda e: e.tensor_scalar(out=r_[:, 176:177], in0=r_[:, 32:33], scalar1=-1.0, scalar2=None,
                                                          op0=ALU.mult), reads=[br], writes=[br])
                    s.op("act", lambda e: e.activation(out=r_[:, 48:52], in_=r_[:, 32:36], func=AF.Exp, bias=r_[:, 176:177],
                                                       scale=1.0, accum_out=r_[:, 52:53]), reads=[br], writes=[br])
                    s.op("dve", lambda e: e.reciprocal(out=r_[:, 53:54], in_=r_[:, 52:53]), reads=[br], writes=[br])
                    s.op("dve", lambda e: e.tensor_scalar(out=GATES[:, 4 * t:4 * t + 4], in0=r_[:, 48:52], scalar1=r_[:, 53:54], scalar2=None,
                                                          op0=ALU.mult), reads=[br], writes=[b_route[t]])
                    s.op("dve", lambda e: e.tensor_scalar(out=r_[:, 64:96], in0=r_[:, 0:32], scalar1=r_[:, 35:36], scalar2=None,
                                                          op0=ALU.is_ge), reads=[br], writes=[br])
                    pf, bpf = pf_rot.next()
                    s.op("pe", lambda e, pf=pf: e.matmul(pf[:, 0:NE], lhsT=ltri, rhs=r_[:, 64:96], start=True, stop=True),
                         reads=[br, b_cst], writes=[bpf])
                    s.op("pe", lambda e, pf=pf: e.matmul(pf[:, 64:64 + NE], lhsT=onesf, rhs=r_[:, 64:96], start=True, stop=True),
                         reads=[br, b_cst], writes=[bpf])
                    s.op("dve", lambda e, pf=pf: e.tensor_tensor(out=r_[:, 96:128], in0=pf[:, 0:NE], in1=BASE[:], op=ALU.add),
                         reads=[bpf, b_BASE], writes=[br])
                    s.op("dve", lambda e, pf=pf: e.tensor_tensor(out=BASE[:], in0=pf[:, 64:64 + NE], in1=BASE[:], op=ALU.add),
                         reads=[bpf, b_BASE], writes=[b_BASE])
                    s.op("dve", lambda e: e.tensor_copy(out=r_[:, 160:164], in_=r_[:, 40:44].bitcast(U32)), reads=[br], writes=[br])
                    for k in range(4):
                        s.op("dve", lambda e, k=k: e.scalar_tensor_tensor(out=r_[:, 128:160], in0=iota32, scalar=r_[:, 160 + k:161 + k],
                                                                          in1=r_[:, 96:128], op0=ALU.is_equal, op1=ALU.mult),
                             reads=[br, b_cst], writes=[br])
                        s.op("dve", lambda e, k=k: e.reduce_sum(out=r_[:, 164 + k:165 + k], in_=r_[:, 128:160], axis=AX.X),
                             reads=[br], writes=[br])
                    s.op("dve", lambda e: e.scalar_tensor_tensor(out=r_[:, 168:172], in0=r_[:, 160:164], scalar=float(cap),
                                                                 in1=r_[:, 164:168], op0=ALU.mult, op1=ALU.add),
                         reads=[br], writes=[br])
                    s.op("dve", lambda e: e.tensor_scalar(out=r_[:, 172:176], in0=r_[:, 164:168], scalar1=float(cap), scalar2=4.0e6,
                                                          op0=ALU.is_ge, op1=ALU.mult), reads=[br], writes=[br])
                    s.op("dve", lambda e: e.tensor_tensor(out=r_[:, 168:172], in0=r_[:, 168:172], in1=r_[:, 172:176], op=ALU.add),
                         reads=[br], writes=[br])
                    s.op("dve", lambda e: e.tensor_copy(out=DEST[:, 4 * t:4 * t + 4], in_=r_[:, 168:172]), reads=[br], writes=[b_route[t]])
                    for k in range(4):
                        s.dma("pool", XG[0:NE * cap, :], hb[i][:], reads=[b_hb[i], b_route[t]], accw=b_XG,
                              indirect=dict(out_offset=bass.IndirectOffsetOnAxis(ap=DEST[:, 4 * t + k:4 * t + k + 1], axis=0), in_offset=None,
                                            bounds_check=bound, oob_is_err=False))
                s.barrier()

            if stop_phase < 4:
                break
            with contextlib.ExitStack() as ph:
                nst = cap // 128
                nsc = (cap + 511) // 512
                scw = cap // nsc
                Gw = sbt(ph, "Gw", [128, KC, FE], BF16); b_Gw = Buf()
                Uw = sbt(ph, "Uw", [128, KC, FE], BF16); b_Uw = Buf()
                Dw = sbt(ph, "Dw", [128, 8, D], BF16); b_Dw = Buf()
                xg = sbt(ph, "xg", [128, nst, D], BF16); b_xg = Buf()
                xgT = sbt(ph, "xgT", [128, KC, cap], BF16); b_xgT = Buf()
                aT = sbt(ph, "aT", [128, 8, cap], BF16); b_aT = Buf()
                bd = sbt(ph, "bd", [128, D], F32); b_bd = Buf()
                bgu = sbt(ph, "bgu", [128, NE * KC], F32); b_bgu = Buf()
                bgl = sbt(ph, "bgl", [128, 4, 128], F32); b_bgl = Buf()
                yst = [sbt(ph, "yst%d" % i, [128, D], F32) for i in range(2)]; b_yst = [Buf(), Buf()]
                y_rot = Rot(list(zip(yst, b_yst)))
                ew = [sbt(ph, "ew%d" % i, [128, 4, scw], F32) for i in range(2)]; b_ew = [Buf(), Buf()]
                ew_rot = Rot(list(zip(ew, b_ew)))
                s.dma("sp", bgl[:], b_gate_up[l].rearrange("e (m p) -> (e m) p", p=128).rearrange("(a q) p -> q a p", q=128),
                      writes=[b_bgl])
                pf, bpf = pf_rot.next()
                for a in range(4):
                    s.op("pe", lambda e, a=a, pf=pf: e.transpose(out=pf[:, a * 128:(a + 1) * 128], in_=bgl[:, a, :], identity=identf),
                         reads=[b_bgl, b_cst], writes=[bpf])
                s.op("dve", lambda e, pf=pf: e.tensor_copy(out=bgu[:], in_=pf[:]), reads=[bpf], writes=[b_bgu])
                for ex in range(NE):
                    s.dma("pool", Gw[:], w_gate_up[l, ex, :, 0:FE].rearrange("(k p) n -> p k n", p=128), writes=[b_Gw])
                    s.dma("pool", Uw[:], w_gate_up[l, ex, :, FE:2 * FE].rearrange("(k p) n -> p k n", p=128), writes=[b_Uw])
                    s.dma("pool", Dw[:], w_down[l, ex].rearrange("(k p) n -> p k n", p=128), writes=[b_Dw])
                    s.dma("sp", bd[:], b_down[l, ex:ex + 1, :].broadcast_to([128, D]), writes=[b_bd])
                    s.dma("sp", xg[:], XG[ex * cap:(ex + 1) * cap, :].rearrange("(j p) n -> p j n", p=128),
                          reads=[b_XG[ex]], writes=[b_xg])
                    for j in range(nst):
                        transpose_bf(lambda i0, cnt, j=j: xgT[:, i0:i0 + cnt, j * 128:(j + 1) * 128],
                                     lambda k, j=j: xg[:, j, k * 128:(k + 1) * 128], KC, [b_xg], lambda i0, cnt: [b_xgT])
                    for m in range(8):
                        for sci in range(nsc):
                            c0 = sci * scw
                            pg, bpg = pf_rot.next()
                            pu, bpu = pf_rot.next()
                            for k in range(KC):
                                s.op("pe", lambda e, k=k, m=m, c0=c0, pg=pg: e.matmul(pg[:, 0:scw], lhsT=Gw[:, k, m * 128:(m + 1) * 128],
                                                                                     rhs=xgT[:, k, c0:c0 + scw], start=(k == 0), stop=(k == KC - 1)),
                                     reads=[b_Gw, b_xgT], writes=[bpg], inc=(k == KC - 1))
                            for k in range(KC):
                                s.op("pe", lambda e, k=k, m=m, c0=c0, pu=pu: e.matmul(pu[:, 0:scw], lhsT=Uw[:, k, m * 128:(m + 1) * 128],
                                                                                     rhs=xgT[:, k, c0:c0 + scw], start=(k == 0), stop=(k == KC - 1)),
                                     reads=[b_Uw, b_xgT], writes=[bpu], inc=(k == KC - 1))
                            w_, bw = ew_rot.next()
                            bg_ap = bgu[:, ex * KC + m:ex * KC + m + 1]
                            bu_ap = bgu[:, ex * KC + 8 + m:ex * KC + 8 + m + 1]
                            s.op("dve", lambda e, pg=pg: e.tensor_scalar(out=w_[:, 0, 0:scw], in0=pg[:, 0:scw], scalar1=bg_ap, scalar2=7.0,
                                                                         op0=ALU.add, op1=ALU.min), reads=[bpg, b_bgu], writes=[bw])
                            s.op("act", lambda e: e.activation(out=w_[:, 1, 0:scw], in_=w_[:, 0, 0:scw], func=AF.Sigmoid, scale=1.702),
                                 reads=[bw], writes=[bw])
                            s.op("dve", lambda e, pu=pu: e.tensor_scalar(out=w_[:, 2, 0:scw], in0=pu[:, 0:scw], scalar1=bu_ap, scalar2=-7.0,
                                                                         op0=ALU.add, op1=ALU.max), reads=[bpu, b_bgu], writes=[bw])
                            s.op("pool", lambda e: e.tensor_scalar(out=w_[:, 2, 0:scw], in0=w_[:, 2, 0:scw], scalar1=7.0, scalar2=1.0,
                                                                   op0=ALU.min, op1=ALU.add), reads=[bw], writes=[bw])
                            s.op("pool", lambda e: e.tensor_tensor(out=w_[:, 3, 0:scw], in0=w_[:, 0, 0:scw], in1=w_[:, 2, 0:scw], op=ALU.mult),
                                 reads=[bw], writes=[bw])
                            s.op("dve", lambda e, m=m, c0=c0: e.tensor_tensor(out=aT[:, m, c0:c0 + scw], in0=w_[:, 3, 0:scw], in1=w_[:, 1, 0:scw],
                                                                             op=ALU.mult), reads=[bw], writes=[b_aT])
                    for j in range(nst):
                        y_t, b_y = y_rot.next()
                        for n4 in range(4):
                            pf, bpf = pf_rot.next()
                            for f in range(8):
                                s.op("pe", lambda e, f=f, j=j, n4=n4, pf=pf: e.matmul(pf[:], lhsT=aT[:, f, j * 128:(j + 1) * 128],
                                                                                     rhs=Dw[:, f, n4 * 512:(n4 + 1) * 512],
                                                                                     start=(f == 0), stop=(f == 7)),
                                     reads=[b_aT, b_Dw], writes=[bpf], inc=(f == 7))
                            s.op("dve", lambda e, n4=n4, pf=pf: e.tensor_tensor(out=y_t[:, n4 * 512:(n4 + 1) * 512], in0=pf[:],
                                                                                in1=bd[:, n4 * 512:(n4 + 1) * 512], op=ALU.add),
                                 reads=[bpf, b_bd], writes=[b_y])
                        s.dma("sp", YG[ex * cap + j * 128:ex * cap + (j + 1) * 128, :], y_t[:], reads=[b_y], accw=[b_YG[ex]])
                s.barrier()

            if stop_phase < 5:
                break
            with contextlib.ExitStack() as ph:
                G2 = sbt(ph, "G2", [128, D], F32); B2 = sbt(ph, "B2", [128, D], F32); b_gb2 = Buf()
                s.dma("sp", G2[:], ln2_g[l:l + 1, :].broadcast_to([128, D]), accw=[b_gb2])
                s.dma("sp", B2[:], ln2_b[l:l + 1, :].broadcast_to([128, D]), accw=[b_gb2])
                NB5 = 2
                gk = [[sbt(ph, "gk%d_%d" % (i, k), [128, D], F32) for k in range(4)] for i in range(NB5)]
                b_gk = [[Buf() for k in range(4)] for i in range(NB5)]
                h5 = [sbt(ph, "h5%d" % i, [128, D], F32) for i in range(NB5)]; b_h5 = [Buf() for _ in range(NB5)]
                o5 = [sbt(ph, "o5%d" % i, [128, D], F32) for i in range(NB5)]; b_o5 = [Buf() for _ in range(NB5)]
                lnt = [(sbt(ph, "st5%d" % i, [128, 4, 6], F32), Buf(), sbt(ph, "mv5%d" % i, [128, 2], F32), Buf(),
                        sbt(ph, "rs5%d" % i, [128, 1], F32), Buf(), sbt(ph, "nm5%d" % i, [128, 1], F32), Buf())
                       for i in range(NB5)]
                for i in range(NB5):
                    for k in range(4):
                        s.op("pool", lambda e, i=i, k=k: e.memset(gk[i][k][:], 0.0), writes=[b_gk[i][k]])
                bound = bound_regs[cap]
                last = (l == n_layers - 1)
                nt_out = nqt
                for t in range(nt_out):
                    i = t % NB5
                    for k in range(4):
                        s.dma("pool", gk[i][k][:], YG[0:NE * cap, :], reads=[b_route[t]] + b_YG, writes=[b_gk[i][k]],
                              indirect=dict(out_offset=None, in_offset=bass.IndirectOffsetOnAxis(ap=DEST[:, 4 * t + k:4 * t + k + 1], axis=0),
                                            bounds_check=bound, oob_is_err=False))
                    s.dma("sp", h5[i][:], HH[t * 128:(t + 1) * 128, :], reads=[b_HH[t]], writes=[b_h5[i]])
                    a = h5[i]
                    s.op("dve", lambda e: e.tensor_scalar(out=a[:], in0=a[:], scalar1=ALPHA, scalar2=None, op0=ALU.mult),
                         reads=[b_h5[i]], writes=[b_h5[i]])
                    for k in range(4):
                        s.op("dve", lambda e, k=k: e.scalar_tensor_tensor(out=a[:], in0=gk[i][k][:], scalar=GATES[:, 4 * t + k:4 * t + k + 1], in1=a[:],
                                                                       op0=ALU.mult, op1=ALU.add),
                             reads=[b_gk[i][k], b_route[t], b_h5[i]], writes=[b_h5[i]])
                    layer_norm(lnt[i], a, b_h5[i], o5[i], b_o5[i], G2, B2, b_gb2)
                    if last:
                        s.dma("sp", out[t * 128:(t + 1) * 128, :], o5[i][:], reads=[b_o5[i]], accw=[b_OUT])
                    else:
                        s.dma("sp", XS[t * 128:(t + 1) * 128, :], o5[i][:], reads=[b_o5[i]], writes=[b_XS[t]])
                s.barrier()
        s.finish()
        print("bass program: %d instrs, %d waits, last-epoch counts %s" % (s.n_ins, s.n_wait, dict(s.ccnt)))
    return nc


def _consts():
    c = np.zeros((128, 416), np.float32)
    c[:, 0:128] = np.eye(128, dtype=np.float32)
    c[:, 128:256] = np.triu(np.ones((128, 128), np.float32), 1)
    c[:, 256:384] = 1.0
    c[:, 384:416] = np.arange(32, dtype=np.float32)[None, :]
    slopes = np.exp2(-8.0 * np.arange(1, 13, dtype=np.float32) / 12.0).astype(np.float32)
    p = np.arange(128)[:, None]
    ca = np.arange(256)[None, :]
    da = np.abs(p - (ca - 64))
    biasA = np.zeros((128, 12, 256), np.float32)
    for h in range(12):
        dil = DIL[h // 4]
        biasA[:, h, :] = np.where(da <= 64, -(slopes[h] * np.float32(dil)) * da.astype(np.float32), np.float32(NEG))
    cb = np.arange(384)[None, :]
    db = np.abs(p - (cb - 128))
    biasB = np.zeros((128, 12, 384), np.float32)
    for h in range(12):
        biasB[:, h, :] = np.where(db <= 128, -slopes[h] * db.astype(np.float32), np.float32(NEG))
    return c, biasA, biasB


_NC_CACHE = {}


def kernel(**inputs):
    x = np.asarray(inputs["x"], dtype=np.float32)
    mem = np.asarray(inputs["mem"], dtype=np.float32)
    c, biasA, biasB = _consts()
    nl = _NC_CACHE.get("n_layers", DEPTH)
    shared = {k: np.ascontiguousarray(np.asarray(v, dtype=np.float32)) for k, v in inputs.items() if k not in ("x", "mem")}
    for k in ("w_gate_up", "w_down"):
        shared[k] = shared[k][:nl]
    shared.update({"consts": c, "biasA": biasA, "biasB": biasB})
    in_maps = []
    for core in range(4):
        m = dict(shared)
        m["x"] = np.ascontiguousarray(x[core])
        m["mem"] = np.ascontiguousarray(mem[core])
        in_maps.append(m)
    if "nc" not in _NC_CACHE:
        _NC_CACHE["nc"] = build_nc()
    res = run_bass_kernel_spmd(_NC_CACHE["nc"], in_maps, core_ids=list(range(4)))
    out = np.empty((4, 4096, D), np.float32)
    for core in range(4):
        out[core] = np.asarray(res.results[core]["out"], dtype=np.float32)
    return out
```

```python
import contextlib
import numpy as np
import concourse.bass as bass
import concourse.mybir as mybir
from concourse.bass_utils import run_bass_kernel_spmd

F32 = mybir.dt.float32
BF16 = mybir.dt.bfloat16
I32 = mybir.dt.int32
U32 = mybir.dt.uint32
ALU = mybir.AluOpType
AF = mybir.ActivationFunctionType
AX = mybir.AxisListType

D = 2048
KC = 16
DEPTH = 4
N_CORES = 8
if N_CORES == 8:
    NQ = [3328, 3200, 2176, 2048]
    NK = [4096, 3328, 3200, 2176]
    CAP = [640, 640, 384, 384]
    OUT_ROWS = 2048
else:
    NQ = [4096, 4096, 4096, 4096]
    NK = [4096, 4096, 4096, 4096]
    CAP = [640, 640, 640, 640]
    OUT_ROWS = 4096
NE = 32
FE = 1024
ALPHA = float(8.0 ** 0.25)
EPS = 1e-5
SCALE = float(128.0 ** -0.5)
DIL = (1, 4, 16)
NEG = -1e30
MAXT = 32


class Buf:
    __slots__ = ("w", "wl", "r", "rp")

    def __init__(self):
        self.w = None
        self.wl = []
        self.r = {}
        self.rp = {}


class Sched:
    def __init__(self, nc, stack, n_dma=(("sp", 32), ("act", 6), ("pool", 32))):
        self.nc = nc
        self.eng = {"pe": nc.tensor, "act": nc.scalar, "dve": nc.vector,
                    "pool": nc.gpsimd, "sp": nc.sync}
        self.csem = {}
        self.ccnt = {}
        self.ckey = {}
        self.stack = stack
        self.new_epoch("e0")
        self.dpool = {}
        self.dnext = {}
        for q, n in n_dma:
            self.dpool[q] = [[stack.enter_context(nc.semaphore("d_%s_%d" % (q, i))), 0,
                              "d_%s_%d" % (q, i)] for i in range(n)]
            self.dnext[q] = 0
        self.waited = {k: {} for k in self.eng}
        self.n_ins = 0
        self.n_wait = 0

    def new_epoch(self, tag):
        for k in ("pe", "act", "dve", "pool"):
            self.ckey[k] = "c_%s_%s" % (k, tag)
            self.csem[k] = self.stack.enter_context(self.nc.semaphore(self.ckey[k]))
            self.ccnt[k] = 0

    def _wait(self, ek, ev):
        if ev is None:
            return
        sem, val, key = ev
        if ek == "pe" and key.startswith("c_pe_"):
            return
        w = self.waited[ek]
        if w.get(key, 0) >= val:
            return
        self.eng[ek].wait_ge(sem, val)
        w[key] = val
        self.n_wait += 1

    def _deps(self, ek, reads, writes, accw):
        for b in reads:
            self._wait(ek, b.w)
            for ev in b.wl:
                self._wait(ek, ev)
        for b in writes:
            self._wait(ek, b.w)
            for ev in b.wl:
                self._wait(ek, ev)
            for ev in b.r.values():
                self._wait(ek, ev)
            for ev in b.rp.values():
                self._wait(ek, ev)
        for b in accw:
            if b.r:
                b.rp = dict(b.r)
                b.r = {}
                b.wl = []
            self._wait(ek, b.w)
            for ev in b.rp.values():
                self._wait(ek, ev)

    def _commit(self, ev, rkey, reads, writes, accw):
        for b in reads:
            b.r[rkey] = ev
        for b in writes:
            b.w = ev
            b.wl = []
            b.r = {}
            b.rp = {}
        for b in accw:
            b.wl.append(ev)

    def op(self, ek, fn, reads=(), writes=(), inc=True):
        self._deps(ek, reads, writes, ())
        ins = fn(self.eng[ek])
        if inc:
            self.ccnt[ek] += 1
            ins.then_inc(self.csem[ek], 1)
            ev = (self.csem[ek], self.ccnt[ek], self.ckey[ek])
        else:
            ev = (self.csem[ek], self.ccnt[ek] + 1, self.ckey[ek])
        self._commit(ev, ek, reads, writes, ())
        self.n_ins += 1
        return ev

    def dma(self, q, out, in_, reads=(), writes=(), accw=(), indirect=None, **kw):
        pool = self.dpool[q]
        slot = pool[self.dnext[q]]
        self.dnext[q] = (self.dnext[q] + 1) % len(pool)
        sem, cnt, key = slot
        self._wait(q, (sem, cnt, key))
        self._deps(q, reads, writes, accw)
        if indirect is None:
            ins = self.eng[q].dma_start(out=out, in_=in_, **kw)
        else:
            ins = self.eng[q].indirect_dma_start(out=out, in_=in_, **indirect)
        ins.then_inc(sem, 16)
        slot[1] = cnt + 16
        ev = (sem, cnt + 16, key)
        self._commit(ev, key, reads, writes, accw)
        self.n_ins += 1
        return ev

    def barrier(self):
        evs = [(self.csem[k], self.ccnt[k], self.ckey[k]) for k in self.csem if self.ccnt[k] > 0]
        for q in self.dpool:
            for sem, cnt, key in self.dpool[q]:
                if cnt > 0:
                    evs.append((sem, cnt, key))
        for ek in self.eng:
            for ev in evs:
                if ev[2] == self.ckey.get(ek):
                    continue
                self._wait(ek, ev)

    def finish(self):
        self.barrier()


class Rot:
    def __init__(self, items):
        self.items = items
        self.i = 0

    def next(self):
        it = self.items[self.i]
        self.i = (self.i + 1) % len(self.items)
        return it


def build_nc(n_layers=DEPTH, stop_phase=99):
    nc = bass.Bass("TRN2", target_bir_lowering=False)

    def din(name, shape, dt=F32):
        return nc.dram_tensor(name, list(shape), dt, kind="ExternalInput").ap()

    x_in = din("x", [4096, D])
    mem_in = din("mem", [256, D])
    w_in_a = din("w_in_a", [2, D, 5120])
    w_in_b = din("w_in_b", [2, D, 2560])
    sink_b = din("sink_b", [2, 12])
    w_mem_kv = din("w_mem_kv", [4, D, 1024])
    w_o = din("w_o", [4, D, D])
    ln1_g = din("ln1_g", [4, D])
    ln1_b = din("ln1_b", [4, D])
    router_w = din("router_w", [4, D, NE])
    router_b = din("router_b", [4, NE])
    w_gate_up = din("w_gate_up", [n_layers, NE, D, 2 * FE])
    b_gate_up = din("b_gate_up", [4, NE, 2 * FE])
    w_down = din("w_down", [n_layers, NE, FE, D])
    b_down = din("b_down", [4, NE, D])
    ln2_g = din("ln2_g", [4, D])
    ln2_b = din("ln2_b", [4, D])
    consts = din("consts", [128, 128 * 3 + 32])
    biasA = din("biasA", [128, 12, 256])
    biasB = din("biasB", [128, 12, 384])
    out = nc.dram_tensor("out", [OUT_ROWS, D], F32, kind="ExternalOutput").ap()

    def dscr(name, shape, dt):
        return nc.dram_tensor(name, list(shape), dt).ap()

    XS = dscr("XS", [4096, D], F32)
    QT = dscr("QT", [16, 128, 4096], BF16)
    KT = dscr("KT", [12, 128, 4096], BF16)
    VV = dscr("VV", [4096, 1536], BF16)
    HEADS = dscr("HEADS", [4096, D], BF16)
    LSE = dscr("LSE", [4096, 12], F32)
    HH = dscr("HH", [4096, D], F32)
    XG = dscr("XG", [NE * 640, D], BF16)
    YG = dscr("YG", [NE * 640, D], F32)

    b_XS = [Buf() for _ in range(32)]
    b_QT = [Buf() for _ in range(16)]
    b_KT = [Buf() for _ in range(12)]
    b_VV = [Buf() for _ in range(32)]
    b_HEADS = [Buf() for _ in range(MAXT)]
    b_LSE = [Buf() for _ in range(MAXT)]
    b_HH = [Buf() for _ in range(MAXT)]
    b_XG = [Buf() for _ in range(NE)]
    b_YG = [Buf() for _ in range(NE)]
    b_OUT = Buf()

    with contextlib.ExitStack() as st:
        s = Sched(nc, st)

        uniq = [0]

        def sbt(stack, name, shape, dt):
            uniq[0] += 1
            return stack.enter_context(nc.sbuf_tensor("%s_%d" % (name, uniq[0]), list(shape), dt))

        cst = sbt(st, "cst", [128, 416], F32); b_cst = Buf()
        identb = sbt(st, "identb", [128, 128], BF16); b_identb = Buf()
        memT = sbt(st, "memT", [128, KC, 256], BF16); b_memT = Buf()
        MKT = sbt(st, "MKT", [128, 4, 256], BF16); b_MKT = Buf()
        MV = sbt(st, "MV", [128, 2, 512], BF16); b_MV = Buf()
        GATES = sbt(st, "GATES", [128, MAXT * 4], F32)
        DEST = sbt(st, "DEST", [128, MAXT * 4], I32)
        b_route = [Buf() for _ in range(MAXT)]
        BASE = sbt(st, "BASE", [128, NE], F32); b_BASE = Buf()
        identf = cst[:, 0:128]
        ltri = cst[:, 128:256]
        onesf = cst[:, 256:384]
        iota32 = cst[:, 384:416]
        PF = [st.enter_context(nc.psum_tensor("PF%d" % i, [128, 512], F32)) for i in range(6)]
        b_PF = [Buf() for _ in range(6)]
        PT = [st.enter_context(nc.psum_tensor("PT%d" % i, [128, 8, 128], BF16)) for i in range(2)]
        b_PT = [Buf() for _ in range(2)]
        pf_rot = Rot(list(zip(PF, b_PF)))
        pt_rot = Rot(list(zip(PT, b_PT)))
        ev_rot = Rot(["act", "dve"])

        bound_regs = {}
        for cp in sorted(set(CAP)):
            rg = nc.gpsimd.alloc_register("bnd%d" % cp)
            nc.gpsimd.reg_mov(rg, NE * cp - 1)
            bound_regs[cp] = rg
        s.dma("sp", cst[:], consts, writes=[b_cst])
        s.op("dve", lambda e: e.tensor_copy(out=identb[:], in_=identf), reads=[b_cst], writes=[b_identb])

        def transpose_bf(dst_fn, src_fn, n, reads, writes_fn):
            i = 0
            while i < n:
                cnt = min(8, n - i)
                pt, bpt = pt_rot.next()
                for j in range(cnt):
                    s.op("pe", lambda e, j=j: e.transpose(out=pt[:, j, :], in_=src_fn(i + j), identity=identb[:]),
                         reads=list(reads) + [b_identb], writes=[bpt])
                ek = ev_rot.next()
                if ek == "act":
                    s.op("act", lambda e: e.copy(out=dst_fn(i, cnt), in_=pt[:, 0:cnt, :]), reads=[bpt], writes=writes_fn(i, cnt))
                else:
                    s.op("dve", lambda e: e.tensor_copy(out=dst_fn(i, cnt), in_=pt[:, 0:cnt, :]), reads=[bpt], writes=writes_fn(i, cnt))
                i += cnt

        with contextlib.ExitStack() as ph:
            mf = sbt(ph, "mf", [128, 2, D], F32); b_mf = Buf()
            mb = sbt(ph, "mb", [128, 2, D], BF16); b_mb = Buf()
            s.dma("sp", mf[:], mem_in.rearrange("(j p) n -> p j n", p=128), writes=[b_mf])
            s.op("dve", lambda e: e.tensor_copy(out=mb[:], in_=mf[:]), reads=[b_mf], writes=[b_mb])
            for j in range(2):
                transpose_bf(lambda i0, cnt, j=j: memT[:, i0:i0 + cnt, j * 128:(j + 1) * 128],
                             lambda i, j=j: mb[:, j, i * 128:(i + 1) * 128], KC, [b_mb], lambda i0, cnt: [b_memT])
            s.barrier()

        def layer_norm(stack_tiles, y, b_y, o, b_o, Gbc, Bbc, b_gb):
            stats, b_stats, mv, b_mv, rstd, b_rstd, nmr, b_nmr = stack_tiles
            for c in range(4):
                s.op("dve", lambda e, c=c: e.bn_stats(out=stats[:, c, :], in_=y[:, c * 512:(c + 1) * 512]),
                     reads=[b_y], writes=[b_stats])
            s.op("dve", lambda e: e.bn_aggr(out=mv[:], in_=stats[:].rearrange("p a b -> p (a b)")), reads=[b_stats], writes=[b_mv])
            s.op("dve", lambda e: e.tensor_scalar(out=rstd[:], in0=mv[:, 1:2], scalar1=EPS, scalar2=None,
                                                  op0=ALU.add), reads=[b_mv], writes=[b_rstd])
            s.op("act", lambda e: e.activation(out=rstd[:], in_=rstd[:], func=AF.Ln), reads=[b_rstd], writes=[b_rstd])
            s.op("act", lambda e: e.activation(out=rstd[:], in_=rstd[:], func=AF.Exp, scale=-0.5), reads=[b_rstd], writes=[b_rstd])
            s.op("dve", lambda e: e.scalar_tensor_tensor(out=nmr[:], in0=mv[:, 0:1], scalar=-1.0, in1=rstd[:],
                                                         op0=ALU.mult, op1=ALU.mult), reads=[b_mv, b_rstd], writes=[b_nmr])
            s.op("act", lambda e: e.activation(out=o[:], in_=y[:], func=AF.Identity, bias=nmr[:], scale=rstd[:]),
                 reads=[b_y, b_rstd, b_nmr], writes=[b_o])
            s.op("pool", lambda e: e.tensor_tensor(out=o[:], in0=o[:], in1=Gbc[:], op=ALU.mult), reads=[b_o, b_gb], writes=[b_o])
            s.op("dve", lambda e: e.tensor_tensor(out=o[:], in0=o[:], in1=Bbc[:], op=ALU.add), reads=[b_o, b_gb], writes=[b_o])

        for l in range(n_layers):
            if l > 0:
                s.new_epoch("e%d" % l)
            is_a = (l % 2 == 0)
            lj = l // 2
            nq, nk, cap = NQ[l], NK[l], CAP[l]
            nqt, nkt = nq // 128, nk // 128
            w_in = w_in_a[lj] if is_a else w_in_b[lj]
            ncols = 5120 if is_a else 2560
            x_src = x_in if l == 0 else XS

            def unit_kind(u):
                if u < 12:
                    return ("q", u)
                if is_a:
                    if u < 24:
                        return ("k", u - 12)
                    if u < 36:
                        return ("v", u - 24)
                    return ("q", 12 + (u - 36))
                if u < 14:
                    return ("k", u - 12)
                if u < 16:
                    return ("v", u - 14)
                return ("q", 12 + (u - 16))

            with contextlib.ExitStack() as ph:
                wm = sbt(ph, "wm", [128, KC, 512], BF16); b_wm = Buf()
                XT = sbt(ph, "XT", [128, KC, 2048], BF16); b_XT = [Buf() for _ in range(16)]
                wg = [sbt(ph, "wg%d" % i, [128, KC, 512], BF16) for i in range(2)]
                b_wg = [Buf() for _ in range(2)]
                wg_rot = Rot(list(zip(wg, b_wg)))
                xf = [sbt(ph, "xf%d" % i, [128, D], F32) for i in range(2)]; b_xf = [Buf(), Buf()]
                xb = [sbt(ph, "xb%d" % i, [128, D], BF16) for i in range(2)]; b_xb = [Buf(), Buf()]
                xf_rot = Rot(list(zip(xf, b_xf, xb, b_xb)))
                ost = [sbt(ph, "ost%d" % i, [128, 512], BF16) for i in range(4)]; b_ost = [Buf() for _ in range(4)]
                ost_rot = Rot(list(zip(ost, b_ost)))

                for half in range(2):
                    s.dma("pool", wm[:], w_mem_kv[l, :, half * 512:(half + 1) * 512].rearrange("(k p) n -> p k n", p=128),
                          writes=[b_wm])
                    if half == 0:
                        for h in range(4):
                            pf, bpf = pf_rot.next()
                            for k in range(KC):
                                s.op("pe", lambda e, k=k, h=h: e.matmul(pf[:, 0:256], lhsT=wm[:, k, h * 128:(h + 1) * 128],
                                                                        rhs=memT[:, k, :], start=(k == 0), stop=(k == KC - 1)),
                                     reads=[b_wm, b_memT], writes=[bpf], inc=(k == KC - 1))
                            s.op("act", lambda e, h=h: e.copy(out=MKT[:, h, :], in_=pf[:, 0:256]), reads=[bpf], writes=[b_MKT])
                    else:
                        for j in range(2):
                            pf, bpf = pf_rot.next()
                            for k in range(KC):
                                s.op("pe", lambda e, k=k, j=j: e.matmul(pf[:], lhsT=memT[:, k, j * 128:(j + 1) * 128],
                                                                        rhs=wm[:, k, :], start=(k == 0), stop=(k == KC - 1)),
                                     reads=[b_wm, b_memT], writes=[bpf], inc=(k == KC - 1))
                            s.op("act", lambda e, j=j: e.copy(out=MV[:, j, :], in_=pf[:]), reads=[bpf], writes=[b_MV])

                for t0 in range(0, nk, 2048):
                    t1 = min(t0 + 2048, nk)
                    ntl = (t1 - t0) // 128
                    for ti in range(ntl):
                        gt = t0 // 128 + ti
                        xft, bxf, xbt, bxb = xf_rot.next()
                        s.dma("sp", xft[:], x_src[gt * 128:(gt + 1) * 128, :], reads=[b_XS[gt]], writes=[bxf])
                        s.op("pool", lambda e: e.tensor_copy(out=xbt[:], in_=xft[:]), reads=[bxf], writes=[bxb])
                        transpose_bf(lambda i0, cnt, ti=ti: XT[:, i0:i0 + cnt, ti * 128:(ti + 1) * 128],
                                     lambda i, xbt=xbt: xbt[:, i * 128:(i + 1) * 128], KC, [bxb],
                                     lambda i0, cnt, ti=ti: [b_XT[ti]])
                    for cg in range(ncols // 512):
                        wgt, bwg = wg_rot.next()
                        s.dma("pool", wgt[:], w_in[:, cg * 512:(cg + 1) * 512].rearrange("(k p) n -> p k n", p=128),
                              writes=[bwg])
                        kinds = [unit_kind(cg * 4 + j) for j in range(4)]
                        for j, (kd, hidx) in enumerate(kinds):
                            if kd == "v":
                                continue
                            lim = nq if kd == "q" else nk
                            dst = QT if kd == "q" else KT
                            bdst = b_QT[hidx] if kd == "q" else b_KT[hidx]
                            for c0 in range(t0, min(t1, lim), 512):
                                c1 = min(c0 + 512, t1, lim)
                                n = c1 - c0
                                pf, bpf = pf_rot.next()
                                tl = [b_XT[(c0 - t0) // 128 + i] for i in range((n + 127) // 128)]
                                for k in range(KC):
                                    s.op("pe", lambda e, k=k, j=j, c0=c0, n=n: e.matmul(
                                        pf[:, 0:n], lhsT=wgt[:, k, j * 128:(j + 1) * 128],
                                        rhs=XT[:, k, c0 - t0:c0 - t0 + n], start=(k == 0), stop=(k == KC - 1)),
                                        reads=[bwg] + tl, writes=[bpf], inc=(k == KC - 1))
                                o_t, b_o = ost_rot.next()
                                ek = ev_rot.next()
                                if ek == "act":
                                    s.op("act", lambda e, n=n: e.copy(out=o_t[:, 0:n], in_=pf[:, 0:n]), reads=[bpf], writes=[b_o])
                                else:
                                    s.op("dve", lambda e, n=n: e.tensor_copy(out=o_t[:, 0:n], in_=pf[:, 0:n]), reads=[bpf], writes=[b_o])
                                s.dma("sp", dst[hidx, :, c0:c1], o_t[:, 0:n], reads=[b_o], accw=[bdst])
                        vj = [j for j, (kd, _) in enumerate(kinds) if kd == "v"]
                        if vj:
                            j0 = vj[0]
                            nv = len(vj) * 128
                            vcol = kinds[j0][1] * 128
                            for ti in range(ntl):
                                gt = t0 // 128 + ti
                                pf, bpf = pf_rot.next()
                                for k in range(KC):
                                    s.op("pe", lambda e, k=k, ti=ti: e.matmul(
                                        pf[:, 0:nv], lhsT=XT[:, k, ti * 128:(ti + 1) * 128],
                                        rhs=wgt[:, k, j0 * 128:j0 * 128 + nv], start=(k == 0), stop=(k == KC - 1)),
                                        reads=[bwg, b_XT[ti]], writes=[bpf], inc=(k == KC - 1))
                                o_t, b_o = ost_rot.next()
                                ek = ev_rot.next()
                                if ek == "act":
                                    s.op("act", lambda e: e.copy(out=o_t[:, 0:nv], in_=pf[:, 0:nv]), reads=[bpf], writes=[b_o])
                                else:
                                    s.op("dve", lambda e: e.tensor_copy(out=o_t[:, 0:nv], in_=pf[:, 0:nv]), reads=[bpf], writes=[b_o])
                                s.dma("sp", VV[gt * 128:(gt + 1) * 128, vcol:vcol + nv], o_t[:, 0:nv], reads=[b_o], accw=[b_VV[gt]])
                s.barrier()

            if stop_phase < 2:
                break
            with contextlib.ExitStack() as ph:
                NB = 3
                sc_t = [sbt(ph, "sc%d" % i, [128, 384], F32) for i in range(NB)]; b_sc = [Buf() for _ in range(NB)]
                p_t = [sbt(ph, "p%d" % i, [128, 384], BF16) for i in range(NB)]; b_p = [Buf() for _ in range(NB)]
                pT_t = [sbt(ph, "pT%d" % i, [128, 3, 128], BF16) for i in range(NB)]; b_pT = [Buf() for _ in range(NB)]
                sm_t = [sbt(ph, "sm%d" % i, [128, 8], F32) for i in range(NB)]; b_sm = [Buf() for _ in range(NB)]
                blk_rot = Rot(list(zip(sc_t, b_sc, p_t, b_p, pT_t, b_pT, sm_t, b_sm)))
                for i in range(NB):
                    s.op("pool", lambda e, i=i: e.memset(p_t[i][:], 0.0), writes=[b_p[i]])

                def attn_block(qT, kT, rq, rk, bias, rb, c_lo, c_hi, chunks, sink, o_dst, b_o, lse_dst, b_lse):
                    sc, bsc, p, bp, pT, bpT, sm, bsm = blk_rot.next()
                    n = c_hi - c_lo
                    pf, bpf = pf_rot.next()
                    s.op("pe", lambda e: e.matmul(pf[:, c_lo:c_hi], lhsT=qT, rhs=kT, start=True, stop=True),
                         reads=[rq, rk], writes=[bpf])
                    if bias is not None:
                        s.op("dve", lambda e: e.scalar_tensor_tensor(out=sc[:, c_lo:c_hi], in0=pf[:, c_lo:c_hi], scalar=SCALE,
                                                                     in1=bias, op0=ALU.mult, op1=ALU.add),
                             reads=[bpf, rb], writes=[bsc])
                        s.op("dve", lambda e: e.reduce_max(out=sm[:, 0:1], in_=sc[:, c_lo:c_hi], axis=AX.X),
                             reads=[bsc], writes=[bsm])
                        if sink is not None:
                            sk_ap, b_sk = sink
                            s.op("dve", lambda e: e.tensor_tensor(out=sm[:, 0:1], in0=sm[:, 0:1], in1=sk_ap, op=ALU.max),
                                 reads=[bsm, b_sk], writes=[bsm])
                        s.op("dve", lambda e: e.tensor_scalar(out=sm[:, 1:2], in0=sm[:, 0:1], scalar1=-1.0, scalar2=None,
                                                              op0=ALU.mult), reads=[bsm], writes=[bsm])
                        s.op("act", lambda e: e.activation(out=p[:, c_lo:c_hi], in_=sc[:, c_lo:c_hi], func=AF.Exp,
                                                           bias=sm[:, 1:2], scale=1.0, accum_out=sm[:, 2:3]),
                             reads=[bsc, bsm], writes=[bp, bsm])
                    else:
                        s.op("dve", lambda e: e.reduce_max(out=sm[:, 0:1], in_=pf[:, c_lo:c_hi], axis=AX.X),
                             reads=[bpf], writes=[bsm])
                        s.op("dve", lambda e: e.tensor_scalar(out=sm[:, 1:2], in0=sm[:, 0:1], scalar1=-SCALE, scalar2=None,
                                                              op0=ALU.mult), reads=[bsm], writes=[bsm])
                        s.op("act", lambda e: e.activation(out=p[:, c_lo:c_hi], in_=pf[:, c_lo:c_hi], func=AF.Exp,
                                                           bias=sm[:, 1:2], scale=SCALE, accum_out=sm[:, 2:3]),
                             reads=[bpf, bsm], writes=[bp, bsm])
                    if sink is not None:
                        sk_ap, b_sk = sink
                        s.op("act", lambda e: e.activation(out=sm[:, 3:4], in_=sk_ap, func=AF.Exp, bias=sm[:, 1:2], scale=1.0),
                             reads=[bsm, b_sk], writes=[bsm])
                        s.op("dve", lambda e: e.tensor_tensor(out=sm[:, 2:3], in0=sm[:, 2:3], in1=sm[:, 3:4], op=ALU.add),
                             reads=[bsm], writes=[bsm])
                    s.op("dve", lambda e: e.reciprocal(out=sm[:, 4:5], in_=sm[:, 2:3]), reads=[bsm], writes=[bsm])
                    if lse_dst is not None:
                        s.op("act", lambda e: e.activation(out=sm[:, 5:6], in_=sm[:, 2:3], func=AF.Ln), reads=[bsm], writes=[bsm])
                        s.op("dve", lambda e: e.tensor_tensor(out=lse_dst, in0=sm[:, 5:6], in1=sm[:, 0:1], op=ALU.add),
                             reads=[bsm], writes=[b_lse])
                    pt, bpt = pt_rot.next()
                    for idx, (ci, rhs, rbufs) in enumerate(chunks):
                        s.op("pe", lambda e, idx=idx, ci=ci: e.transpose(out=pt[:, idx, :], in_=p[:, ci * 128:(ci + 1) * 128],
                                                                         identity=identb[:]),
                             reads=[bp, b_identb], writes=[bpt])
                    nch = len(chunks)
                    s.op("act", lambda e: e.copy(out=pT[:, 0:nch, :], in_=pt[:, 0:nch, :]), reads=[bpt], writes=[bpT])
                    pf2, bpf2 = pf_rot.next()
                    for idx, (ci, rhs, rbufs) in enumerate(chunks):
                        s.op("pe", lambda e, idx=idx, rhs=rhs: e.matmul(pf2[:, 0:128], lhsT=pT[:, idx, :], rhs=rhs,
                                                                        start=(idx == 0), stop=(idx == nch - 1)),
                             reads=[bpT] + list(rbufs), writes=[bpf2], inc=(idx == nch - 1))
                    s.op("dve", lambda e: e.tensor_scalar(out=o_dst, in0=pf2[:, 0:128], scalar1=sm[:, 4:5], scalar2=None,
                                                          op0=ALU.mult), reads=[bpf2, bsm], writes=[b_o])

                def fix_p_zero(c_lo, c_hi, width):
                    for i in range(NB):
                        if c_lo > 0:
                            s.op("pool", lambda e, i=i: e.memset(p_t[i][:, 0:c_lo], 0.0), writes=[b_p[i]])
                        if c_hi < width:
                            s.op("pool", lambda e, i=i: e.memset(p_t[i][:, c_hi:width], 0.0), writes=[b_p[i]])

                with contextlib.ExitStack() as ph2:
                    qm = sbt(ph2, "qm", [128, 4, nq], BF16); b_qm = Buf()
                    for h in range(4):
                        s.dma("sp", qm[:, h, :], QT[12 + h, :, 0:nq], reads=[b_QT[12 + h]], writes=[b_qm])
                    ost2 = [sbt(ph2, "mo%d" % i, [128, 512], BF16) for i in range(2)]; b_ost2 = [Buf(), Buf()]
                    o_rot = Rot(list(zip(ost2, b_ost2)))
                    for blk in range(nqt):
                        o_t, b_o = o_rot.next()
                        for h in range(4):
                            chunks = [(ci, MV[:, ci, h * 128:(h + 1) * 128], [b_MV]) for ci in range(2)]
                            attn_block(qm[:, h, blk * 128:(blk + 1) * 128], MKT[:, h, :], b_qm, b_MKT, None, None,
                                       0, 256, chunks, None, o_t[:, h * 128:(h + 1) * 128], b_o, None, None)
                        s.dma("sp", HEADS[blk * 128:(blk + 1) * 128, 1536:2048], o_t[:], reads=[b_o], accw=[b_HEADS[blk]])
                    s.barrier()

                if is_a:
                    for g in range(3):
                        d = DIL[g]
                        lq, lk = nq // d, nk // d
                        with contextlib.ExitStack() as ph2:
                            bt = sbt(ph2, "bt", [128, 4, 256], F32); b_bt = Buf()
                            s.dma("sp", bt[:], biasA[:, 4 * g:4 * g + 4, :], writes=[b_bt])
                            kt = sbt(ph2, "kt", [128, 4, nk], BF16); b_kt = Buf()
                            qt = sbt(ph2, "qt", [128, 4, nq], BF16); b_qt = Buf()
                            for hh in range(4):
                                s.dma("sp", kt[:, hh, :], KT[4 * g + hh, :, 0:nk], reads=[b_KT[4 * g + hh]], writes=[b_kt])
                                s.dma("sp", qt[:, hh, :], QT[4 * g + hh, :, 0:nq], reads=[b_QT[4 * g + hh]], writes=[b_qt])
                            vch = [sbt(ph2, "vch%d" % i, [128, 512], BF16) for i in range(6)]; b_vch = [Buf() for _ in range(6)]
                            for i in range(6):
                                s.op("pool", lambda e, i=i: e.memset(vch[i][:], 0.0), writes=[b_vch[i]])
                            v_rot = Rot(list(zip(vch, b_vch)))
                            ost2 = [sbt(ph2, "ao%d" % i, [128, 512], BF16) for i in range(2)]; b_ost2 = [Buf(), Buf()]
                            lst2 = [sbt(ph2, "al%d" % i, [128, 4], F32) for i in range(2)]; b_lst2 = [Buf(), Buf()]
                            o_rot = Rot(list(zip(ost2, b_ost2, lst2, b_lst2)))
                            starts = list(range(0, lq - 127, 128))
                            if lq % 128:
                                starts.append(lq - 128)
                            p_state = (0, 256)
                            for r in range(d):
                                for i0 in starts:
                                    j0 = i0 - 64
                                    c_lo = max(0, -j0)
                                    c_hi = min(256, lk - j0)
                                    if (c_lo, c_hi) != p_state:
                                        fix_p_zero(c_lo, c_hi, 256)
                                        p_state = (c_lo, c_hi)
                                    chunks_v = []
                                    for ci in range(2):
                                        jb = j0 + 128 * ci
                                        p_lo = max(0, -jb)
                                        p_hi = min(128, lk - jb)
                                        if p_hi <= p_lo:
                                            continue
                                        vt, bv = v_rot.next()
                                        tok0 = r + d * (jb + p_lo)
                                        cnt = p_hi - p_lo
                                        tok_last = tok0 + d * (cnt - 1)
                                        tiles = sorted(set(range(tok0 // 128, tok_last // 128 + 1)))
                                        s.dma("act", vt[p_lo:p_hi, :], VV[tok0:tok_last + 1:d, 512 * g:512 * g + 512],
                                              reads=[b_VV[t] for t in tiles], writes=[bv])
                                        chunks_v.append((ci, vt, bv))
                                    o_t, b_o, l_t, b_l = o_rot.next()
                                    q0 = r + d * i0
                                    qlast = q0 + d * 127
                                    k0 = r + d * (j0 + c_lo)
                                    klast = r + d * (j0 + c_hi - 1)
                                    for hh in range(4):
                                        chunks = [(ci, vt[:, hh * 128:(hh + 1) * 128], [bv]) for (ci, vt, bv) in chunks_v]
                                        attn_block(qt[:, hh, q0:qlast + 1:d], kt[:, hh, k0:klast + 1:d], b_qt, b_kt,
                                                   bt[:, hh, c_lo:c_hi], b_bt, c_lo, c_hi, chunks, None,
                                                   o_t[:, hh * 128:(hh + 1) * 128], b_o, l_t[:, hh:hh + 1], b_l)
                                    tiles = sorted(set(range(q0 // 128, qlast // 128 + 1)))
                                    s.dma("sp", HEADS[q0:qlast + 1:d, 512 * g:512 * g + 512], o_t[:], reads=[b_o],
                                          accw=[b_HEADS[t] for t in tiles])
                                    s.dma("sp", LSE[q0:qlast + 1:d, 4 * g:4 * g + 4], l_t[:], reads=[b_l],
                                          accw=[b_LSE[t] for t in tiles])
                            if p_state != (0, 256):
                                fix_p_zero(0, 256, 256)
                            s.barrier()
                else:
                    with contextlib.ExitStack() as ph2:
                        bt = sbt(ph2, "btb", [128, 12, 384], F32); b_bt = Buf()
                        s.dma("sp", bt[:], biasB, writes=[b_bt])
                        sk = sbt(ph2, "sk", [128, 12], F32); b_sk = Buf()
                        s.dma("sp", sk[:], sink_b[lj:lj + 1, :].broadcast_to([128, 12]), writes=[b_sk])
                        kt = sbt(ph2, "ktb", [128, 2, nk], BF16); b_kt = Buf()
                        for kv in range(2):
                            s.dma("sp", kt[:, kv, :], KT[kv, :, 0:nk], reads=[b_KT[kv]], writes=[b_kt])
                        vs = sbt(ph2, "vsb", [128, nkt, 256], BF16); b_vs = Buf()
                        s.dma("sp", vs[:], VV[0:nk, 0:256].rearrange("(j p) n -> p j n", p=128),
                              reads=[b_VV[t] for t in range(nkt)], writes=[b_vs])
                        qt = sbt(ph2, "qtb", [128, 6, nq], BF16); b_qt = Buf()
                        ost2 = [sbt(ph2, "bo%d" % i, [128, 768], BF16) for i in range(2)]; b_ost2 = [Buf(), Buf()]
                        o_rot = Rot(list(zip(ost2, b_ost2)))
                        pb_state = [(0, 384)]
                        for kv in range(2):
                            for hh in range(6):
                                s.dma("sp", qt[:, hh, :], QT[6 * kv + hh, :, 0:nq], reads=[b_QT[6 * kv + hh]], writes=[b_qt])
                            for blk in range(nqt):
                                i0 = blk * 128
                                c_lo = 128 if blk == 0 else 0
                                c_hi = min(384, nk - i0 + 128)
                                if (c_lo, c_hi) != pb_state[0]:
                                    fix_p_zero(c_lo, c_hi, 384)
                                    pb_state[0] = (c_lo, c_hi)
                                o_t, b_o = o_rot.next()
                                for hh in range(6):
                                    h = 6 * kv + hh
                                    chunks = [(ci, vs[:, blk - 1 + ci, kv * 128:(kv + 1) * 128], [b_vs])
                                              for ci in range(3) if 0 <= blk - 1 + ci < nkt]
                                    attn_block(qt[:, hh, i0:i0 + 128], kt[:, kv, i0 - 128 + c_lo:i0 - 128 + c_hi], b_qt, b_kt,
                                               bt[:, h, c_lo:c_hi], b_bt, c_lo, c_hi, chunks, (sk[:, h:h + 1], b_sk),
                                               o_t[:, hh * 128:(hh + 1) * 128], b_o, None, None)
                                s.dma("sp", HEADS[i0:i0 + 128, 768 * kv:768 * kv + 768], o_t[:], reads=[b_o],
                                      accw=[b_HEADS[blk]])
                        s.barrier()

            if stop_phase < 3:
                break
            with contextlib.ExitStack() as ph:
                wo = sbt(ph, "wo", [128, KC, D], BF16); b_wo = Buf()
                for c in range(4):
                    s.dma("pool", wo[:, :, c * 512:(c + 1) * 512],
                          w_o[l, :, c * 512:(c + 1) * 512].rearrange("(k p) n -> p k n", p=128), accw=[b_wo])
                G1 = sbt(ph, "G1", [128, D], F32); B1 = sbt(ph, "B1", [128, D], F32); b_gb = Buf()
                s.dma("sp", G1[:], ln1_g[l:l + 1, :].broadcast_to([128, D]), accw=[b_gb])
                s.dma("sp", B1[:], ln1_b[l:l + 1, :].broadcast_to([128, D]), accw=[b_gb])
                rw = sbt(ph, "rw", [128, KC, NE], F32); b_rw = Buf()
                s.dma("sp", rw[:], router_w[l].rearrange("(k p) n -> p k n", p=128), writes=[b_rw])
                rb = sbt(ph, "rb", [128, NE], F32); b_rb = Buf()
                s.dma("sp", rb[:], router_b[l:l + 1, :].broadcast_to([128, NE]), writes=[b_rb])
                s.op("pool", lambda e: e.memset(BASE[:], 0.0), writes=[b_BASE])
                NB3 = 2
                hd = [sbt(ph, "hd%d" % i, [128, D], BF16) for i in range(NB3)]; b_hd = [Buf() for _ in range(NB3)]
                ls = [sbt(ph, "ls%d" % i, [128, 12], F32) for i in range(NB3)]; b_ls = [Buf() for _ in range(NB3)]
                mw = [sbt(ph, "mw%d" % i, [128, 24], F32) for i in range(NB3)]; b_mw = [Buf() for _ in range(NB3)]
                hT = [sbt(ph, "hT%d" % i, [128, KC, 128], BF16) for i in range(NB3)]; b_hT = [Buf() for _ in range(NB3)]
                xr = [sbt(ph, "xr%d" % i, [128, D], F32) for i in range(NB3)]; b_xr = [Buf() for _ in range(NB3)]
                yy = [sbt(ph, "yy%d" % i, [128, D], F32) for i in range(NB3)]; b_yy = [Buf() for _ in range(NB3)]
                hh_t = [sbt(ph, "hh%d" % i, [128, D], F32) for i in range(NB3)]; b_hh = [Buf() for _ in range(NB3)]
                hb = [sbt(ph, "hb%d" % i, [128, D], BF16) for i in range(NB3)]; b_hb = [Buf() for _ in range(NB3)]
                hTf = [sbt(ph, "hTf%d" % i, [128, KC, 128], F32) for i in range(1)]; b_hTf = [Buf()]
                lnt = [(sbt(ph, "st%d" % i, [128, 4, 6], F32), Buf(), sbt(ph, "mv%d" % i, [128, 2], F32), Buf(),
                        sbt(ph, "rs%d" % i, [128, 1], F32), Buf(), sbt(ph, "nm%d" % i, [128, 1], F32), Buf())
                       for i in range(NB3)]
                rt = [sbt(ph, "rt%d" % i, [128, 256], F32) for i in range(NB3)]; b_rt = [Buf() for _ in range(NB3)]
                bound = bound_regs[cap]
                for t in range(nqt):
                    i = t % NB3
                    s.dma("sp", hd[i][:], HEADS[t * 128:(t + 1) * 128, :], reads=[b_HEADS[t]], writes=[b_hd[i]])
                    s.dma("sp", xr[i][:], x_src[t * 128:(t + 1) * 128, :], reads=[b_XS[t]], writes=[b_xr[i]])
                    if is_a:
                        s.dma("sp", ls[i][:], LSE[t * 128:(t + 1) * 128, :], reads=[b_LSE[t]], writes=[b_ls[i]])
                        m = mw[i]
                        bm = b_mw[i]
                        s.op("dve", lambda e: e.tensor_tensor(out=m[:, 0:4], in0=ls[i][:, 0:4], in1=ls[i][:, 4:8], op=ALU.max),
                             reads=[b_ls[i]], writes=[bm])
                        s.op("dve", lambda e: e.tensor_tensor(out=m[:, 0:4], in0=m[:, 0:4], in1=ls[i][:, 8:12], op=ALU.max),
                             reads=[b_ls[i], bm], writes=[bm])
                        for g in range(3):
                            s.op("dve", lambda e, g=g: e.tensor_tensor(out=m[:, 4 + 4 * g:8 + 4 * g], in0=ls[i][:, 4 * g:4 * g + 4],
                                                                       in1=m[:, 0:4], op=ALU.subtract),
                                 reads=[b_ls[i], bm], writes=[bm])
                        s.op("act", lambda e: e.activation(out=m[:, 4:16], in_=m[:, 4:16], func=AF.Exp), reads=[bm], writes=[bm])
                        s.op("dve", lambda e: e.tensor_tensor(out=m[:, 16:20], in0=m[:, 4:8], in1=m[:, 8:12], op=ALU.add),
                             reads=[bm], writes=[bm])
                        s.op("dve", lambda e: e.tensor_tensor(out=m[:, 16:20], in0=m[:, 16:20], in1=m[:, 12:16], op=ALU.add),
                             reads=[bm], writes=[bm])
                        s.op("dve", lambda e: e.reciprocal(out=m[:, 20:24], in_=m[:, 16:20]), reads=[bm], writes=[bm])
                        for g in range(3):
                            s.op("dve", lambda e, g=g: e.tensor_tensor(out=m[:, 4 + 4 * g:8 + 4 * g], in0=m[:, 4 + 4 * g:8 + 4 * g],
                                                                       in1=m[:, 20:24], op=ALU.mult), reads=[bm], writes=[bm])
                        for hx in range(12):
                            ek = "pool" if hx % 2 else "dve"
                            s.op(ek, lambda e, hx=hx: e.tensor_scalar(out=hd[i][:, hx * 128:(hx + 1) * 128],
                                                                      in0=hd[i][:, hx * 128:(hx + 1) * 128],
                                                                      scalar1=m[:, 4 + hx:5 + hx], scalar2=None, op0=ALU.mult),
                                 reads=[b_hd[i], bm], writes=[b_hd[i]])
                    transpose_bf(lambda i0, cnt: hT[i][:, i0:i0 + cnt, :], lambda k: hd[i][:, k * 128:(k + 1) * 128], KC,
                                 [b_hd[i]], lambda i0, cnt: [b_hT[i]])
                    pfs = [pf_rot.next() for _ in range(4)]
                    for c in range(4):
                        pf, bpf = pfs[c]
                        for k in range(KC):
                            s.op("pe", lambda e, k=k, c=c, pf=pf: e.matmul(pf[:], lhsT=hT[i][:, k, :], rhs=wo[:, k, c * 512:(c + 1) * 512],
                                                                           start=(k == 0), stop=(k == KC - 1)),
                                 reads=[b_hT[i], b_wo], writes=[bpf], inc=(k == KC - 1))
                    for c in range(4):
                        pf, bpf = pfs[c]
                        s.op("dve", lambda e, c=c, pf=pf: e.scalar_tensor_tensor(out=yy[i][:, c * 512:(c + 1) * 512],
                                                                                 in0=xr[i][:, c * 512:(c + 1) * 512], scalar=ALPHA,
                                                                                 in1=pf[:], op0=ALU.mult, op1=ALU.add),
                             reads=[b_xr[i], bpf], writes=[b_yy[i]])
                    layer_norm(lnt[i], yy[i], b_yy[i], hh_t[i], b_hh[i], G1, B1, b_gb)
                    s.dma("sp", HH[t * 128:(t + 1) * 128, :], hh_t[i][:], reads=[b_hh[i]], writes=[b_HH[t]])
                    s.op("act", lambda e: e.copy(out=hb[i][:], in_=hh_t[i][:]), reads=[b_hh[i]], writes=[b_hb[i]])
                    for k4 in range(4):
                        pf, bpf = pf_rot.next()
                        for j in range(4):
                            k = k4 * 4 + j
                            s.op("pe", lambda e, k=k, j=j, pf=pf: e.transpose(out=pf[:, j * 128:(j + 1) * 128],
                                                                              in_=hh_t[i][:, k * 128:(k + 1) * 128], identity=identf),
                                 reads=[b_hh[i], b_cst], writes=[bpf])
                        s.op("act", lambda e, k4=k4, pf=pf: e.copy(out=hTf[0][:, k4 * 4:(k4 + 1) * 4, :],
                                                                   in_=pf[:].rearrange("p (a b) -> p a b", a=4)),
                             reads=[bpf], writes=[b_hTf[0]])
                    pf, bpf = pf_rot.next()
                    for k in range(KC):
                        s.op("pe", lambda e, k=k, pf=pf: e.matmul(pf[:, 0:NE], lhsT=hTf[0][:, k, :], rhs=rw[:, k, :],
                                                                  start=(k == 0), stop=(k == KC - 1)),
                             reads=[b_hTf[0], b_rw], writes=[bpf], inc=(k == KC - 1))
                    r_ = rt[i]
                    br = b_rt[i]
                    s.op("dve", lambda e, pf=pf: e.tensor_tensor(out=r_[:, 0:32], in0=pf[:, 0:NE], in1=rb[:], op=ALU.add),
                         reads=[bpf, b_rb], writes=[br])
                    s.op("dve", lambda e: e.max(out=r_[:, 32:40], in_=r_[:, 0:32]), reads=[br], writes=[br])
                    s.op("dve", lambda e: e.max_index(out=r_[:, 40:48].bitcast(U32), in_max=r_[:, 32:40], in_values=r_[:, 0:32]),
                         reads=[br], writes=[br])
                    s.op("dve", lambda e: e.tensor_scalar(out=r_[:, 176:177], in0=r_[:, 32:33], scalar1=-1.0, scalar2=None,
                                                          op0=ALU.mult), reads=[br], writes=[br])
                    s.op("act", lambda e: e.activation(out=r_[:, 48:52], in_=r_[:, 32:36], func=AF.Exp, bias=r_[:, 176:177],
                                                       scale=1.0, accum_out=r_[:, 52:53]), reads=[br], writes=[br])
                    s.op("dve", lambda e: e.reciprocal(out=r_[:, 53:54], in_=r_[:, 52:53]), reads=[br], writes=[br])
                    s.op("dve", lambda e: e.tensor_scalar(out=GATES[:, 4 * t:4 * t + 4], in0=r_[:, 48:52], scalar1=r_[:, 53:54], scalar2=None,
                                                          op0=ALU.mult), reads=[br], writes=[b_route[t]])
                    s.op("dve", lambda e: e.tensor_scalar(out=r_[:, 64:96], in0=r_[:, 0:32], scalar1=r_[:, 35:36], scalar2=None,
                                                          op0=ALU.is_ge), reads=[br], writes=[br])
                    pf, bpf = pf_rot.next()
                    s.op("pe", lambda e, pf=pf: e.matmul(pf[:, 0:NE], lhsT=ltri, rhs=r_[:, 64:96], start=True, stop=True),
                         reads=[br, b_cst], writes=[bpf])
                    s.op("pe", lambda e, pf=pf: e.matmul(pf[:, 64:64 + NE], lhsT=onesf, rhs=r_[:, 64:96], start=True, stop=True),
                         reads=[br, b_cst], writes=[bpf])
                    s.op("dve", lambda e, pf=pf: e.tensor_tensor(out=r_[:, 96:128], in0=pf[:, 0:NE], in1=BASE[:], op=ALU.add),
                         reads=[bpf, b_BASE], writes=[br])
                    s.op("dve", lambda e, pf=pf: e.tensor_tensor(out=BASE[:], in0=pf[:, 64:64 + NE], in1=BASE[:], op=ALU.add),
                         reads=[bpf, b_BASE], writes=[b_BASE])
                    s.op("dve", lambda e: e.tensor_copy(out=r_[:, 160:164], in_=r_[:, 40:44].bitcast(U32)), reads=[br], writes=[br])
                    for k in range(4):
                        s.op("dve", lambda e, k=k: e.scalar_tensor_tensor(out=r_[:, 128:160], in0=iota32, scalar=r_[:, 160 + k:161 + k],
                                                                          in1=r_[:, 96:128], op0=ALU.is_equal, op1=ALU.mult),
                             reads=[br, b_cst], writes=[br])
                        s.op("dve", lambda e, k=k: e.reduce_sum(out=r_[:, 164 + k:165 + k], in_=r_[:, 128:160], axis=AX.X),
                             reads=[br], writes=[br])
                    s.op("dve", lambda e: e.scalar_tensor_tensor(out=r_[:, 168:172], in0=r_[:, 160:164], scalar=float(cap),
                                                                 in1=r_[:, 164:168], op0=ALU.mult, op1=ALU.add),
                         reads=[br], writes=[br])
                    s.op("dve", lambda e: e.tensor_scalar(out=r_[:, 172:176], in0=r_[:, 164:168], scalar1=float(cap), scalar2=4.0e6,
                                                          op0=ALU.is_ge, op1=ALU.mult), reads=[br], writes=[br])
                    s.op("dve", lambda e: e.tensor_tensor(out=r_[:, 168:172], in0=r_[:, 168:172], in1=r_[:, 172:176], op=ALU.add),
                         reads=[br], writes=[br])
                    s.op("dve", lambda e: e.tensor_copy(out=DEST[:, 4 * t:4 * t + 4], in_=r_[:, 168:172]), reads=[br], writes=[b_route[t]])
                    for k in range(4):
                        s.dma("pool", XG[0:NE * cap, :], hb[i][:], reads=[b_hb[i], b_route[t]], accw=b_XG,
                              indirect=dict(out_offset=bass.IndirectOffsetOnAxis(ap=DEST[:, 4 * t + k:4 * t + k + 1], axis=0), in_offset=None,
                                            bounds_check=bound, oob_is_err=False))
                s.barrier()

            if stop_phase < 4:
                break
            with contextlib.ExitStack() as ph:
                nst = cap // 128
                nsc = (cap + 511) // 512
                scw = cap // nsc
                Gw = sbt(ph, "Gw", [128, KC, FE], BF16); b_Gw = Buf()
                Uw = sbt(ph, "Uw", [128, KC, FE], BF16); b_Uw = Buf()
                Dw = sbt(ph, "Dw", [128, 8, D], BF16); b_Dw = Buf()
                xg = sbt(ph, "xg", [128, nst, D], BF16); b_xg = Buf()
                xgT = sbt(ph, "xgT", [128, KC, cap], BF16); b_xgT = Buf()
                aT = sbt(ph, "aT", [128, 8, cap], BF16); b_aT = Buf()
                bd = sbt(ph, "bd", [128, D], F32); b_bd = Buf()
                bgu = sbt(ph, "bgu", [128, NE * KC], F32); b_bgu = Buf()
                bgl = sbt(ph, "bgl", [128, 4, 128], F32); b_bgl = Buf()
                yst = [sbt(ph, "yst%d" % i, [128, D], F32) for i in range(2)]; b_yst = [Buf(), Buf()]
                y_rot = Rot(list(zip(yst, b_yst)))
                ew = [sbt(ph, "ew%d" % i, [128, 4, scw], F32) for i in range(2)]; b_ew = [Buf(), Buf()]
                ew_rot = Rot(list(zip(ew, b_ew)))
                s.dma("sp", bgl[:], b_gate_up[l].rearrange("e (m p) -> (e m) p", p=128).rearrange("(a q) p -> q a p", q=128),
                      writes=[b_bgl])
                pf, bpf = pf_rot.next()
                for a in range(4):
                    s.op("pe", lambda e, a=a, pf=pf: e.transpose(out=pf[:, a * 128:(a + 1) * 128], in_=bgl[:, a, :], identity=identf),
                         reads=[b_bgl, b_cst], writes=[bpf])
                s.op("dve", lambda e, pf=pf: e.tensor_copy(out=bgu[:], in_=pf[:]), reads=[bpf], writes=[b_bgu])
                for ex in range(NE):
                    s.dma("pool", Gw[:], w_gate_up[l, ex, :, 0:FE].rearrange("(k p) n -> p k n", p=128), writes=[b_Gw])
                    s.dma("pool", Uw[:], w_gate_up[l, ex, :, FE:2 * FE].rearrange("(k p) n -> p k n", p=128), writes=[b_Uw])
                    s.dma("pool", Dw[:], w_down[l, ex].rearrange("(k p) n -> p k n", p=128), writes=[b_Dw])
                    s.dma("sp", bd[:], b_down[l, ex:ex + 1, :].broadcast_to([128, D]), writes=[b_bd])
                    s.dma("sp", xg[:], XG[ex * cap:(ex + 1) * cap, :].rearrange("(j p) n -> p j n", p=128),
                          reads=[b_XG[ex]], writes=[b_xg])
                    for j in range(nst):
                        transpose_bf(lambda i0, cnt, j=j: xgT[:, i0:i0 + cnt, j * 128:(j + 1) * 128],
                                     lambda k, j=j: xg[:, j, k * 128:(k + 1) * 128], KC, [b_xg], lambda i0, cnt: [b_xgT])
                    for m in range(8):
                        for sci in range(nsc):
                            c0 = sci * scw
                            pg, bpg = pf_rot.next()
                            pu, bpu = pf_rot.next()
                            for k in range(KC):
                                s.op("pe", lambda e, k=k, m=m, c0=c0, pg=pg: e.matmul(pg[:, 0:scw], lhsT=Gw[:, k, m * 128:(m + 1) * 128],
                                                                                     rhs=xgT[:, k, c0:c0 + scw], start=(k == 0), stop=(k == KC - 1)),
                                     reads=[b_Gw, b_xgT], writes=[bpg], inc=(k == KC - 1))
                            for k in range(KC):
                                s.op("pe", lambda e, k=k, m=m, c0=c0, pu=pu: e.matmul(pu[:, 0:scw], lhsT=Uw[:, k, m * 128:(m + 1) * 128],
                                                                                     rhs=xgT[:, k, c0:c0 + scw], start=(k == 0), stop=(k == KC - 1)),
                                     reads=[b_Uw, b_xgT], writes=[bpu], inc=(k == KC - 1))
                            w_, bw = ew_rot.next()
                            bg_ap = bgu[:, ex * KC + m:ex * KC + m + 1]
                            bu_ap = bgu[:, ex * KC + 8 + m:ex * KC + 8 + m + 1]
                            s.op("dve", lambda e, pg=pg: e.tensor_scalar(out=w_[:, 0, 0:scw], in0=pg[:, 0:scw], scalar1=bg_ap, scalar2=7.0,
                                                                         op0=ALU.add, op1=ALU.min), reads=[bpg, b_bgu], writes=[bw])
                            s.op("act", lambda e: e.activation(out=w_[:, 1, 0:scw], in_=w_[:, 0, 0:scw], func=AF.Sigmoid, scale=1.702),
                                 reads=[bw], writes=[bw])
                            s.op("dve", lambda e, pu=pu: e.tensor_scalar(out=w_[:, 2, 0:scw], in0=pu[:, 0:scw], scalar1=bu_ap, scalar2=-7.0,
                                                                         op0=ALU.add, op1=ALU.max), reads=[bpu, b_bgu], writes=[bw])
                            s.op("pool", lambda e: e.tensor_scalar(out=w_[:, 2, 0:scw], in0=w_[:, 2, 0:scw], scalar1=7.0, scalar2=1.0,
                                                                   op0=ALU.min, op1=ALU.add), reads=[bw], writes=[bw])
                            s.op("pool", lambda e: e.tensor_tensor(out=w_[:, 3, 0:scw], in0=w_[:, 0, 0:scw], in1=w_[:, 2, 0:scw], op=ALU.mult),
                                 reads=[bw], writes=[bw])
                            s.op("dve", lambda e, m=m, c0=c0: e.tensor_tensor(out=aT[:, m, c0:c0 + scw], in0=w_[:, 3, 0:scw], in1=w_[:, 1, 0:scw],
                                                                             op=ALU.mult), reads=[bw], writes=[b_aT])
                    for j in range(nst):
                        y_t, b_y = y_rot.next()
                        for n4 in range(4):
                            pf, bpf = pf_rot.next()
                            for f in range(8):
                                s.op("pe", lambda e, f=f, j=j, n4=n4, pf=pf: e.matmul(pf[:], lhsT=aT[:, f, j * 128:(j + 1) * 128],
                                                                                     rhs=Dw[:, f, n4 * 512:(n4 + 1) * 512],
                                                                                     start=(f == 0), stop=(f == 7)),
                                     reads=[b_aT, b_Dw], writes=[bpf], inc=(f == 7))
                            s.op("dve", lambda e, n4=n4, pf=pf: e.tensor_tensor(out=y_t[:, n4 * 512:(n4 + 1) * 512], in0=pf[:],
                                                                                in1=bd[:, n4 * 512:(n4 + 1) * 512], op=ALU.add),
                                 reads=[bpf, b_bd], writes=[b_y])
                        s.dma("sp", YG[ex * cap + j * 128:ex * cap + (j + 1) * 128, :], y_t[:], reads=[b_y], accw=[b_YG[ex]])
                s.barrier()

            if stop_phase < 5:
                break
            with contextlib.ExitStack() as ph:
                G2 = sbt(ph, "G2", [128, D], F32); B2 = sbt(ph, "B2", [128, D], F32); b_gb2 = Buf()
                s.dma("sp", G2[:], ln2_g[l:l + 1, :].broadcast_to([128, D]), accw=[b_gb2])
                s.dma("sp", B2[:], ln2_b[l:l + 1, :].broadcast_to([128, D]), accw=[b_gb2])
                NB5 = 2
                gk = [[sbt(ph, "gk%d_%d" % (i, k), [128, D], F32) for k in range(4)] for i in range(NB5)]
                b_gk = [[Buf() for k in range(4)] for i in range(NB5)]
                h5 = [sbt(ph, "h5%d" % i, [128, D], F32) for i in range(NB5)]; b_h5 = [Buf() for _ in range(NB5)]
                o5 = [sbt(ph, "o5%d" % i, [128, D], F32) for i in range(NB5)]; b_o5 = [Buf() for _ in range(NB5)]
                lnt = [(sbt(ph, "st5%d" % i, [128, 4, 6], F32), Buf(), sbt(ph, "mv5%d" % i, [128, 2], F32), Buf(),
                        sbt(ph, "rs5%d" % i, [128, 1], F32), Buf(), sbt(ph, "nm5%d" % i, [128, 1], F32), Buf())
                       for i in range(NB5)]
                for i in range(NB5):
                    for k in range(4):
                        s.op("pool", lambda e, i=i, k=k: e.memset(gk[i][k][:], 0.0), writes=[b_gk[i][k]])
                bound = bound_regs[cap]
                last = (l == n_layers - 1)
                nt_out = min(nqt, OUT_ROWS // 128) if last else nqt
                for t in range(nt_out):
                    i = t % NB5
                    for k in range(4):
                        s.dma("pool", gk[i][k][:], YG[0:NE * cap, :], reads=[b_route[t]] + b_YG, writes=[b_gk[i][k]],
                              indirect=dict(out_offset=None, in_offset=bass.IndirectOffsetOnAxis(ap=DEST[:, 4 * t + k:4 * t + k + 1], axis=0),
                                            bounds_check=bound, oob_is_err=False))
                    s.dma("sp", h5[i][:], HH[t * 128:(t + 1) * 128, :], reads=[b_HH[t]], writes=[b_h5[i]])
                    a = h5[i]
                    s.op("dve", lambda e: e.tensor_scalar(out=a[:], in0=a[:], scalar1=ALPHA, scalar2=None, op0=ALU.mult),
                         reads=[b_h5[i]], writes=[b_h5[i]])
                    for k in range(4):
                        s.op("dve", lambda e, k=k: e.scalar_tensor_tensor(out=a[:], in0=gk[i][k][:], scalar=GATES[:, 4 * t + k:4 * t + k + 1], in1=a[:],
                                                                       op0=ALU.mult, op1=ALU.add),
                             reads=[b_gk[i][k], b_route[t], b_h5[i]], writes=[b_h5[i]])
                    layer_norm(lnt[i], a, b_h5[i], o5[i], b_o5[i], G2, B2, b_gb2)
                    if last:
                        s.dma("sp", out[t * 128:(t + 1) * 128, :], o5[i][:], reads=[b_o5[i]], accw=[b_OUT])
                    else:
                        s.dma("sp", XS[t * 128:(t + 1) * 128, :], o5[i][:], reads=[b_o5[i]], writes=[b_XS[t]])
                s.barrier()
        s.finish()
        print("bass program: %d instrs, %d waits, last-epoch counts %s" % (s.n_ins, s.n_wait, dict(s.ccnt)))
    return nc


def _consts():
    c = np.zeros((128, 416), np.float32)
    c[:, 0:128] = np.eye(128, dtype=np.float32)
    c[:, 128:256] = np.triu(np.ones((128, 128), np.float32), 1)
    c[:, 256:384] = 1.0
    c[:, 384:416] = np.arange(32, dtype=np.float32)[None, :]
    slopes = np.exp2(-8.0 * np.arange(1, 13, dtype=np.float32) / 12.0).astype(np.float32)
    p = np.arange(128)[:, None]
    ca = np.arange(256)[None, :]
    da = np.abs(p - (ca - 64))
    biasA = np.zeros((128, 12, 256), np.float32)
    for h in range(12):
        dil = DIL[h // 4]
        biasA[:, h, :] = np.where(da <= 64, -(slopes[h] * np.float32(dil)) * da.astype(np.float32), np.float32(NEG))
    cb = np.arange(384)[None, :]
    db = np.abs(p - (cb - 128))
    biasB = np.zeros((128, 12, 384), np.float32)
    for h in range(12):
        biasB[:, h, :] = np.where(db <= 128, -slopes[h] * db.astype(np.float32), np.float32(NEG))
    return c, biasA, biasB


_NC_CACHE = {}


def kernel(**inputs):
    x = np.asarray(inputs["x"], dtype=np.float32)
    mem = np.asarray(inputs["mem"], dtype=np.float32)
    c, biasA, biasB = _consts()
    nl = _NC_CACHE.get("n_layers", DEPTH)
    shared = {k: np.ascontiguousarray(np.asarray(v, dtype=np.float32)) for k, v in inputs.items() if k not in ("x", "mem")}
    for k in ("w_gate_up", "w_down"):
        shared[k] = shared[k][:nl]
    shared.update({"consts": c, "biasA": biasA, "biasB": biasB})
    in_maps = []
    for core in range(N_CORES):
        m = dict(shared)
        if N_CORES == 8:
            b, half = core // 2, core % 2
            xs = x[b] if half == 0 else x[b, ::-1]
        else:
            b, xs = core, x[core]
        m["x"] = np.ascontiguousarray(xs)
        m["mem"] = np.ascontiguousarray(mem[b])
        in_maps.append(m)
    if "nc" not in _NC_CACHE:
        _NC_CACHE["nc"] = build_nc()
    res = run_bass_kernel_spmd(_NC_CACHE["nc"], in_maps, core_ids=list(range(N_CORES)))
    out = np.empty((4, 4096, D), np.float32)
    for core in range(N_CORES):
        y = np.asarray(res.results[core]["out"], dtype=np.float32)
        if N_CORES == 8:
            b, half = core // 2, core % 2
            if half == 0:
                out[b, 0:2048] = y
            else:
                out[b, 2048:4096] = y[::-1]
        else:
            out[core] = y
    return out
```
